# Optimizing a Trainium2 kernel written in Bass

```python
import jax, jax.numpy as jnp
from jax import lax
import numpy as np

D_MODEL = 2048
BATCH = 4
SEQ = 2048
DEPTH = 2

CHUNK = 64
N_HEADS = 16
HEAD_DIM = 128
N_KV_HEADS = 4
HEADS_PER_KV = N_HEADS // N_KV_HEADS
IDX_HEADS = 16
IDX_DIM = 64
TOPK_MAX = 256
ROPE_THETA = 10000.0
POOL_WINDOWS = (2, 4, 8, 16)
POOL_W = D_MODEL
POOL_GROUP = POOL_W // len(POOL_WINDOWS)
LRU_W = D_MODEL
LRU_BLOCKS = 16
LRU_BLOCK = LRU_W // LRU_BLOCKS
CONV_WIDTH = 4
LRU_C = 8.0
ATTN_W = N_HEADS * HEAD_DIM
KV_W = N_KV_HEADS * HEAD_DIM
MIX_W = ATTN_W + POOL_W + LRU_W
IN_SPLITS = (ATTN_W, KV_W, KV_W, IDX_HEADS * IDX_DIM, IDX_DIM, IDX_HEADS,
             POOL_W, LRU_W, LRU_W, MIX_W)
IN_COLS = sum(IN_SPLITS)
IN_OFFSETS = tuple(int(v) for v in np.cumsum(IN_SPLITS)[:-1])
D_FF = 5504
N_EXPERTS = 8
TOP_K = 2
D_EXPERT = 7168
MOE_BLOCK = 128
N_DENSE = (DEPTH + 1) // 2
N_MOE = DEPTH // 2
ALPHA = (2 * DEPTH) ** 0.25
BETA = (8 * DEPTH) ** -0.25
LN_EPS = 1e-5

kernel_name = "chunk_causal_hybrid_dsa_pool_rglru_moe"


def layer_norm(x, g, b):
    xf = x.astype(jnp.float32)
    mu = jnp.mean(xf, axis=-1, keepdims=True)
    var = jnp.mean(jnp.square(xf - mu), axis=-1, keepdims=True)
    y = (xf - mu) * lax.rsqrt(var + LN_EPS) * g.astype(jnp.float32) + b.astype(jnp.float32)
    return y.astype(x.dtype)


def rope_tables(positions, dim):
    inv = ROPE_THETA ** (-jnp.arange(0, dim, 2, dtype=jnp.float32) / dim)
    ang = positions.astype(jnp.float32)[..., None] * inv
    return jnp.cos(ang)[:, :, None, :], jnp.sin(ang)[:, :, None, :]


def apply_rope(x, cos, sin):
    half = x.shape[-1] // 2
    x1 = x[..., :half].astype(jnp.float32)
    x2 = x[..., half:].astype(jnp.float32)
    return jnp.concatenate([x1 * cos - x2 * sin, x2 * cos + x1 * sin], axis=-1).astype(x.dtype)


def dsa_attention(q, k, v, qi, ki, wi):
    B, S = q.shape[0], q.shape[1]
    n_blk = S // CHUNK
    topk = min(TOPK_MAX, S // 4)
    key_pos = jnp.arange(S)
    wi = wi * (IDX_HEADS ** -0.5)

    def to_blocks(t):
        return jnp.swapaxes(t.reshape((B, n_blk, CHUNK) + t.shape[2:]), 0, 1)

    def one_chunk(args):
        q_b, qi_b, wi_b, blk = args
        limit = (blk + 1) * CHUNK
        s_idx = jnp.einsum('bqhd,bsd->bqhs', qi_b, ki) * (IDX_DIM ** -0.5)
        score = jnp.einsum('bqh,bqhs->bqs', wi_b.astype(jnp.float32),
                           jax.nn.relu(s_idx).astype(jnp.float32))
        score = jnp.where(key_pos < limit, score, -jnp.inf)
        _, sel = lax.top_k(score, topk)
        valid = sel < limit
        k_sel = jax.vmap(lambda kb, ib: kb[ib])(k, sel)
        v_sel = jax.vmap(lambda vb, ib: vb[ib])(v, sel)
        qg = q_b.reshape(B, CHUNK, N_KV_HEADS, HEADS_PER_KV, HEAD_DIM)
        logits = jnp.einsum('bqgrd,bqkgd->bqgrk', qg, k_sel).astype(jnp.float32) * (HEAD_DIM ** -0.5)
        logits = jnp.where(valid[:, :, None, None, :], logits, -jnp.inf)
        p = jax.nn.softmax(logits, axis=-1).astype(v.dtype)
        o = jnp.einsum('bqgrk,bqkgd->bqgrd', p, v_sel)
        return o.reshape(B, CHUNK, ATTN_W)

    out = lax.map(one_chunk, (to_blocks(q), to_blocks(qi), to_blocks(wi), jnp.arange(n_blk)))
    return jnp.swapaxes(out, 0, 1).reshape(B, S, ATTN_W)


def pool_mixer(p, pool_w, pool_scale):
    B, S, _ = p.shape
    pg = p.reshape(B, S, len(POOL_WINDOWS), POOL_GROUP)
    pf = pg.astype(jnp.float32)
    csum = jnp.cumsum(pf, axis=1)
    t = jnp.arange(S)
    means = []
    for g, win in enumerate(POOL_WINDOWS):
        c = csum[:, :, g]
        prev = jnp.pad(c, ((0, 0), (win, 0), (0, 0)))[:, :S]
        cnt = jnp.minimum(t + 1, win).astype(jnp.float32)[None, :, None]
        means.append((c - prev) / cnt)
    diff = (jnp.stack(means, axis=2) - pf).astype(p.dtype)
    y = jnp.einsum('bsgc,gcd->bsgd', diff, pool_w).reshape(B, S, POOL_W)
    return y * pool_scale


def rglru_mixer(xr, gr, conv_w, conv_b, wa, ba, wx, bx, lam):
    B, S, _ = xr.shape
    xp = jnp.pad(xr, ((0, 0), (CONV_WIDTH - 1, 0), (0, 0)))
    xc = conv_b
    for tap in range(CONV_WIDTH):
        xc = xc + xp[:, tap:tap + S] * conv_w[tap]
    xb = xc.reshape(B, S, LRU_BLOCKS, LRU_BLOCK)
    r = jax.nn.sigmoid(jnp.einsum('bsni,nij->bsnj', xb, wa).reshape(B, S, LRU_W) + ba)
    i = jax.nn.sigmoid(jnp.einsum('bsni,nij->bsnj', xb, wx).reshape(B, S, LRU_W) + bx)
    log_a = -LRU_C * r.astype(jnp.float32) * jax.nn.softplus(-lam.astype(jnp.float32))
    a = jnp.exp(log_a)
    b_in = jnp.sqrt(-jnp.expm1(2.0 * log_a)) * (i * xc).astype(jnp.float32)

    def combine(left, right):
        a1, b1 = left
        a2, b2 = right
        return a1 * a2, a2 * b1 + b2

    _, h = lax.associative_scan(combine, (a, b_in), axis=1)
    return h.astype(xr.dtype) * jax.nn.gelu(gr)


def hybrid_mixer(x, cos_a, sin_a, cos_i, sin_i, w_in, w_out, pool_w, pool_scale,
                 conv_w, conv_b, wa, ba, wx, bx, lam):
    B, S, _ = x.shape
    proj = jnp.einsum('bsd,dc->bsc', x, w_in)
    q, k, v, qi, ki, wi, p, xr, gr, gates = jnp.split(proj, IN_OFFSETS, axis=-1)
    q = apply_rope(q.reshape(B, S, N_HEADS, HEAD_DIM), cos_a, sin_a)
    k = apply_rope(k.reshape(B, S, N_KV_HEADS, HEAD_DIM), cos_a, sin_a)
    v = v.reshape(B, S, N_KV_HEADS, HEAD_DIM)
    qi = apply_rope(qi.reshape(B, S, IDX_HEADS, IDX_DIM), cos_i, sin_i)
    ki = apply_rope(ki[:, :, None, :], cos_i, sin_i)[:, :, 0]
    y_a = dsa_attention(q, k, v, qi, ki, wi)
    y_b = pool_mixer(p, pool_w, pool_scale)
    y_c = rglru_mixer(xr, gr, conv_w, conv_b, wa, ba, wx, bx, lam)
    merged = jax.nn.sigmoid(gates) * jnp.concatenate([y_a, y_b, y_c], axis=-1)
    return jnp.einsum('bsc,cd->bsd', merged, w_out)


def swiglu(x, w_gate, w_up, w_down):
    h = jax.nn.silu(jnp.einsum('bsd,df->bsf', x, w_gate)) * jnp.einsum('bsd,df->bsf', x, w_up)
    return jnp.einsum('bsf,fd->bsd', h, w_down)


def moe_swiglu(x, w_router, w_gate, w_up, w_down):
    B, S, D = x.shape
    xt = x.reshape(-1, D)
    n = xt.shape[0]
    logits = jnp.einsum('nd,de->ne', xt.astype(jnp.float32), w_router.astype(jnp.float32))
    top_logit, top_e = lax.top_k(logits, TOP_K)
    top_w = jax.nn.softmax(top_logit, axis=-1)
    n_assign = n * TOP_K
    flat_e = top_e.reshape(-1)
    flat_w = top_w.reshape(-1)
    flat_tok = jnp.repeat(jnp.arange(n, dtype=jnp.int32), TOP_K)
    order = jnp.argsort(flat_e)
    se, st, sw = flat_e[order], flat_tok[order], flat_w[order]
    counts = jnp.bincount(flat_e, length=N_EXPERTS)
    padded = (counts + MOE_BLOCK - 1) // MOE_BLOCK * MOE_BLOCK
    start_sorted = jnp.cumsum(counts) - counts
    end_padded = jnp.cumsum(padded)
    start_padded = end_padded - padded
    dest = start_padded[se] + jnp.arange(n_assign) - start_sorted[se]
    n_rows = n_assign + N_EXPERTS * MOE_BLOCK
    n_blocks = n_rows // MOE_BLOCK
    row_tok = jnp.zeros((n_rows,), jnp.int32).at[dest].set(st)
    row_w = jnp.zeros((n_rows,), jnp.float32).at[dest].set(sw)
    blk_e = jnp.minimum(jnp.searchsorted(end_padded, jnp.arange(n_blocks) * MOE_BLOCK, side='right'),
                        N_EXPERTS - 1)
    xb = xt[row_tok].reshape(n_blocks, MOE_BLOCK, D)

    def expert_rows(args):
        xblk, e = args
        h = jax.nn.silu(xblk @ w_gate[e]) * (xblk @ w_up[e])
        return h @ w_down[e]

    yb = lax.map(expert_rows, (xb, blk_e)).reshape(n_rows, D)
    y = jnp.zeros((n, D), jnp.float32).at[row_tok].add(yb.astype(jnp.float32) * row_w[:, None])
    return y.astype(x.dtype).reshape(B, S, D)


def setup_inputs(seed: int = 0) -> dict:
    key = jax.random.key(seed)
    ks = jax.random.split(key, 26)
    f32 = jnp.float32

    def nrm(k, shape, scale):
        return jax.random.normal(k, shape, f32) * scale

    x = jax.random.normal(ks[0], (BATCH, SEQ, D_MODEL), f32)
    offsets = jax.random.randint(ks[1], (BATCH, 1), 0, 64) * CHUNK
    positions = (offsets + jnp.arange(SEQ, dtype=jnp.int32)[None, :]).astype(jnp.int32)
    u = jax.random.uniform(ks[12], (DEPTH, LRU_W), f32, minval=0.9, maxval=0.999)
    s_base = u ** (1.0 / LRU_C)
    lru_lam = jnp.log(s_base) - jnp.log1p(-s_base)
    return {
        "x": x,
        "positions": positions,
        "mix_w_in": nrm(ks[2], (DEPTH, D_MODEL, IN_COLS), D_MODEL ** -0.5),
        "mix_w_out": nrm(ks[3], (DEPTH, MIX_W, D_MODEL), BETA * MIX_W ** -0.5),
        "pool_w": nrm(ks[4], (DEPTH, len(POOL_WINDOWS), POOL_GROUP, POOL_GROUP), POOL_GROUP ** -0.5),
        "pool_scale": 1.0 + nrm(ks[5], (DEPTH, POOL_W), 0.02),
        "conv_w": nrm(ks[6], (DEPTH, CONV_WIDTH, LRU_W), CONV_WIDTH ** -0.5),
        "conv_b": nrm(ks[7], (DEPTH, LRU_W), 0.02),
        "lru_wa": nrm(ks[8], (DEPTH, LRU_BLOCKS, LRU_BLOCK, LRU_BLOCK), LRU_BLOCK ** -0.5),
        "lru_ba": nrm(ks[9], (DEPTH, LRU_W), 0.02),
        "lru_wx": nrm(ks[10], (DEPTH, LRU_BLOCKS, LRU_BLOCK, LRU_BLOCK), LRU_BLOCK ** -0.5),
        "lru_bx": nrm(ks[11], (DEPTH, LRU_W), 0.02),
        "lru_lam": lru_lam,
        "ln_mix_g": 1.0 + nrm(ks[13], (DEPTH, D_MODEL), 0.02),
        "ln_mix_b": nrm(ks[14], (DEPTH, D_MODEL), 0.02),
        "ln_ffn_g": 1.0 + nrm(ks[15], (DEPTH, D_MODEL), 0.02),
        "ln_ffn_b": nrm(ks[16], (DEPTH, D_MODEL), 0.02),
        "dense_w_gate": nrm(ks[17], (N_DENSE, D_MODEL, D_FF), D_MODEL ** -0.5),
        "dense_w_up": nrm(ks[18], (N_DENSE, D_MODEL, D_FF), D_MODEL ** -0.5),
        "dense_w_down": nrm(ks[19], (N_DENSE, D_FF, D_MODEL), BETA * D_FF ** -0.5),
        "moe_router": nrm(ks[20], (N_MOE, D_MODEL, N_EXPERTS), D_MODEL ** -0.5),
        "moe_w_gate": nrm(ks[21], (N_MOE, N_EXPERTS, D_MODEL, D_EXPERT), D_MODEL ** -0.5),
        "moe_w_up": nrm(ks[22], (N_MOE, N_EXPERTS, D_MODEL, D_EXPERT), D_MODEL ** -0.5),
        "moe_w_down": nrm(ks[23], (N_MOE, N_EXPERTS, D_EXPERT, D_MODEL), BETA * D_EXPERT ** -0.5),
    }


def reference(x, positions, mix_w_in, mix_w_out, pool_w, pool_scale, conv_w, conv_b,
              lru_wa, lru_ba, lru_wx, lru_bx, lru_lam, ln_mix_g, ln_mix_b, ln_ffn_g, ln_ffn_b,
              dense_w_gate, dense_w_up, dense_w_down, moe_router, moe_w_gate, moe_w_up, moe_w_down):
    cos_a, sin_a = rope_tables(positions, HEAD_DIM)
    cos_i, sin_i = rope_tables(positions, IDX_DIM)
    for layer in range(DEPTH):
        m = hybrid_mixer(x, cos_a, sin_a, cos_i, sin_i, mix_w_in[layer], mix_w_out[layer],
                         pool_w[layer], pool_scale[layer], conv_w[layer], conv_b[layer],
                         lru_wa[layer], lru_ba[layer], lru_wx[layer], lru_bx[layer], lru_lam[layer])
        x = layer_norm(ALPHA * x + m, ln_mix_g[layer], ln_mix_b[layer])
        if layer % 2 == 0:
            j = layer // 2
            f = swiglu(x, dense_w_gate[j], dense_w_up[j], dense_w_down[j])
        else:
            j = layer // 2
            f = moe_swiglu(x, moe_router[j], moe_w_gate[j], moe_w_up[j], moe_w_down[j])
        x = layer_norm(ALPHA * x + f, ln_ffn_g[layer], ln_ffn_b[layer])
    return x
```

```python
import contextlib
import numpy as np
import ml_dtypes
import concourse.bass as bass
import concourse.mybir as mybir
from concourse.bass_utils import run_bass_kernel_spmd

F32 = mybir.dt.float32
BF16 = mybir.dt.bfloat16
I32 = mybir.dt.int32
AF = mybir.ActivationFunctionType
ALU = mybir.AluOpType
AX = mybir.AxisListType

FULL = dict(D=2048, T=2048, B=4, L=2, H=16, G=4, HI=16, TOPK=256, DFF=5504, NE=8, DEXP=7168, NCORES=8, SPLIT=2)
LN_EPS = 1e-5
LIM_SEM = 24000
NEG1 = -1.0e30
NEG2 = -3.0e38
MAGIC = 12582912.0
TWO_PI = 6.283185307179586
C1 = 6.28125
C2 = TWO_PI - 6.28125


class TB:
    __slots__ = ("w", "weng", "r")

    def __init__(self):
        self.w = {}
        self.weng = None
        self.r = {}


class Ctx:
    def __init__(self, nc, st):
        self.nc = nc
        self.st = st
        self.nsem = 0
        self.LIM = LIM_SEM
        self.eng = {"pe": nc.tensor, "act": nc.scalar, "dve": nc.vector, "pool": nc.gpsimd, "sp": nc.sync}
        self.sem = {}
        self.cnt = {}
        self.seen = {k: {} for k in self.eng}
        for k in ("pe", "act", "dve", "pool"):
            self.sem[k] = st.enter_context(nc.semaphore("s_" + k))
            self.cnt[k] = 0
        self.NS = 8
        self.dsem = {}
        self.di = {}
        for q in ("sp", "pool", "act"):
            self.dsem[q] = [st.enter_context(nc.semaphore("d_%s%d" % (q, i))) for i in range(self.NS)]
            self.di[q] = 0
        self.semid = {}
        self.last_out = []

    def _wait(self, e, ev):
        if ev is None:
            return
        sem, val = ev
        k = id(sem)
        if self.seen[e].get(k, 0) >= val:
            return
        self.eng[e].wait_ge(sem, val)
        self.seen[e][k] = val

    def _deps(self, e, R, W, acc_ok=False):
        for b in R:
            for ev in b.w.values():
                self._wait(e, ev)
        for b in W:
            for k, ev in b.w.items():
                if not (acc_ok and k == "pe"):
                    self._wait(e, ev)
            for ev in b.r.values():
                self._wait(e, ev)

    def _mark(self, key, e, ev, R, W):
        for b in R:
            b.r[key] = ev
        for b in W:
            b.w[key] = ev
            b.r = {}

    def op(self, e, fn, R=(), W=(), acc_ok=False):
        self._deps(e, R, W, acc_ok)
        ins = fn()
        self.cnt[e] += 1
        ins.then_inc(self.sem[e], 1)
        ev = (self.sem[e], self.cnt[e])
        self._mark(e, e, ev, R, W)
        return ev

    def barrier(self):
        evs = [(self.sem[k], self.cnt[k]) for k in ("pe", "act", "dve", "pool") if self.cnt[k] > 0]
        for q in self.dsem:
            di = self.di[q]
            for j in range(self.NS):
                if di > j:
                    evs.append((self.dsem[q][j], 16 * ((di - j - 1) // self.NS + 1)))
        for e in ("pe", "act", "dve", "pool", "sp"):
            for ev in evs:
                self._wait(e, ev)
        for k in ("pe", "act", "dve", "pool"):
            if self.cnt[k] > self.LIM:
                self.nsem += 1
                self.sem[k] = self.st.enter_context(self.nc.semaphore("s_%s_%d" % (k, self.nsem)))
                self.cnt[k] = 0
        for q in self.dsem:
            if 16 * (self.di[q] // self.NS + 1) > self.LIM:
                self.nsem += 1
                self.dsem[q] = [self.st.enter_context(self.nc.semaphore("d_%s%d_%d" % (q, i, self.nsem)))
                                for i in range(self.NS)]
                self.di[q] = 0

    def dma(self, q, out, in_, R=(), W=(), **kw):
        self._deps(q, R, W)
        i = self.di[q]
        sem = self.dsem[q][i % self.NS]
        if i >= self.NS:
            self._wait(q, (sem, 16 * (i // self.NS)))
        ins = self.eng[q].dma_start(out=out, in_=in_, **kw)
        ins.then_inc(sem, 16)
        ev = (sem, 16 * (i // self.NS + 1))
        self.di[q] = i + 1
        self._mark("dma_%s_%d" % (q, i % self.NS), "dma", ev, R, W)
        return ev


def build_nc(cfg):
    D, T, L, H, G, HI = cfg["D"], cfg["T"], cfg["L"], cfg["H"], cfg["G"], cfg["HI"]
    TOPK, DFF, NE, DEXP = cfg["TOPK"], cfg["DFF"], cfg["NE"], cfg["DEXP"]
    SPLIT = cfg.get("SPLIT", 1)
    NSEQ = cfg["B"] * SPLIT // cfg["NCORES"]
    assert SPLIT == 1 or NSEQ == 1
    KC, NT, NB = D // 128, T // 128, T // 512
    TQL = T // SPLIT
    HPG = H // G
    AW, KW, IW = H * 128, G * 128, HI * 64
    MIXW = AW + 2 * D
    O_Q, O_K, O_V = 0, AW, AW + KW
    O_QI = AW + 2 * KW
    O_KI = O_QI + IW
    O_WI = O_KI + 64
    O_P = O_WI + HI
    O_XR = O_P + D
    O_GR = O_XR + D
    O_GT = O_GR + D
    INC = O_GT + MIXW
    NMC = MIXW // 128
    NAC = AW // 128
    ALPHA = (2 * L) ** 0.25
    NR = TOPK // 8
    PW = D // 4

    nc = bass.Bass("TRN2", target_bir_lowering=False)

    def din(name, shape, dt=F32):
        return nc.dram_tensor(name, list(shape), dt, kind="ExternalInput").ap()

    x_in = din("x", [NSEQ, T, D])
    pos_in = din("positions", [NSEQ, T], I32)
    posq_in = din("positions_q", [NSEQ, TQL], I32)
    sel_in = din("c_sel", [128, 2])
    limf_in = din("c_limf", [128, NT])
    limq_in = din("c_limq", [128, NT])
    w_in = din("mix_w_in", [L, D, INC])
    w_out = din("mix_w_out", [L, MIXW, D])
    pool_w = din("pool_w", [L, 4, PW, PW])
    pool_scale = din("pool_scale", [L, D])
    conv_w = din("conv_w", [L, 4, D])
    conv_b = din("conv_b", [L, D])
    lru_wa = din("lru_wa", [L, KC, 128, 128])
    lru_ba = din("lru_ba", [L, D])
    lru_wx = din("lru_wx", [L, KC, 128, 128])
    lru_bx = din("lru_bx", [L, D])
    lru_lam = din("lru_lam", [L, D])
    ln_mix_g = din("ln_mix_g", [L, D])
    ln_mix_b = din("ln_mix_b", [L, D])
    ln_ffn_g = din("ln_ffn_g", [L, D])
    ln_ffn_b = din("ln_ffn_b", [L, D])
    dw_gate = din("dense_w_gate", [(L + 1) // 2, D, DFF])
    dw_up = din("dense_w_up", [(L + 1) // 2, D, DFF])
    dw_down = din("dense_w_down", [(L + 1) // 2, DFF, D])
    routerT = din("moe_routerT", [max(L // 2, 1), NE, D])
    mw_gate = din("moe_w_gate", [max(L // 2, 1), NE, D, DEXP])
    mw_up = din("moe_w_up", [max(L // 2, 1), NE, D, DEXP])
    mw_down = din("moe_w_down", [max(L // 2, 1), NE, DEXP, D])
    ident_in = din("c_ident", [128, 128])
    inva_in = din("c_inva", [128, 64])
    invi_in = din("c_invi", [128, 32])
    y_out = nc.dram_tensor("y", [NSEQ, TQL, D], F32, kind="ExternalOutput").ap()

    DBG = cfg.get("STOP")

    def dscr(name, shape, dt):
        if DBG and name in ("xres_d", "pre_d", "yf_d", "yT_d", "qT_d", "qiT_d"):
            return nc.dram_tensor(name, list(shape), dt, kind="ExternalOutput").ap()
        return nc.dram_tensor(name, list(shape), dt, kind="Internal").ap()

    xres_d = dscr("xres_d", [T, D], F32)
    pre_d = dscr("pre_d", [T, D], F32)
    yf_d = dscr("yf_d", [T, D], F32)
    yT_d = dscr("yT_d", [NMC, 128, T], BF16)
    mT_d = dscr("mT_d", [NT, 128, NMC, 128], BF16)
    qT_d = dscr("qT_d", [NT, 128, H, 128], BF16)
    qiT_d = dscr("qiT_d", [NT, 128, HI // 2, 128], BF16)
    tb_xres = [TB() for _ in range(NT)]
    tb_pre = [TB() for _ in range(NT)]
    tb_yf = [TB() for _ in range(NT)]
    tb_yT = [TB() for _ in range(NMC)]
    tb_mT = TB()
    tb_qT = [TB() for _ in range(NT)]
    tb_qiT = [TB() for _ in range(NT)]

    class _Stop(Exception):
        pass

    with contextlib.ExitStack() as gst:
        cx = Ctx(nc, gst)
        stopf = [False]

        @contextlib.contextmanager
        def scope():
            with contextlib.ExitStack() as st_:
                yield st_
                cx.barrier()

        uid = [0]

        def sb(st, name, shape, dt):
            uid[0] += 1
            return st.enter_context(nc.sbuf_tensor("%s_%d" % (name, uid[0]), list(shape), dt)), TB()

        def pp(st, name, shape, dt):
            return st.enter_context(nc.psum_tensor(name, list(shape), dt)), TB()

        V = lambda fn, R=(), W=(): cx.op("dve", fn, R, W)
        A = lambda fn, R=(), W=(): cx.op("act", fn, R, W)
        P = lambda fn, R=(), W=(): cx.op("pool", fn, R, W)

        def MM(out, lhsT, rhs, start, stop, R, W):
            return cx.op("pe", lambda: nc.tensor.matmul(out, lhsT, rhs, start=start, stop=stop), R, W, acc_ok=not start)

        def TR(out, in_, ident, R, W):
            return cx.op("pe", lambda: nc.tensor.transpose(out, in_, ident), R, W, acc_ok=True)

        def act(out, in_, func, R, W, **kw):
            return A(lambda: nc.scalar.activation(out=out, in_=in_, func=func, **kw), R, W)

        def vts(out, in0, s1, s2, op0, op1, R, W):
            if op1 is None:
                return V(lambda: nc.vector.tensor_scalar(out=out, in0=in0, scalar1=s1, scalar2=None, op0=op0), R, W)
            return V(lambda: nc.vector.tensor_scalar(out=out, in0=in0, scalar1=s1, scalar2=s2, op0=op0, op1=op1), R, W)

        def vtt(out, in0, in1, op, R, W):
            return V(lambda: nc.vector.tensor_tensor(out=out, in0=in0, in1=in1, op=op), R, W)

        def vstt(out, in0, sc, in1, op0, op1, R, W):
            return V(lambda: nc.vector.scalar_tensor_tensor(out=out, in0=in0, scalar=sc, in1=in1, op0=op0, op1=op1), R, W)

        def wload(dst, dtb, src2d, rows, cols):
            nk = rows // 128
            for k0 in range(0, nk, 4):
                k1 = min(nk, k0 + 4)
                cx.dma("pool", dst[:, k0:k1, 0:cols],
                       src2d[k0 * 128:k1 * 128, :].rearrange("(k p) n -> p k n", p=128), W=[dtb])

        def bcast_row(dst, dtb, src_row, n):
            cx.dma("sp", dst, src_row.partition_broadcast(128), W=[dtb])

        ident_f, tb_idf = sb(gst, "ident_f", [128, 128], F32)
        ident, tb_id = sb(gst, "ident", [128, 128], BF16)
        inva, tb_inva = sb(gst, "inva", [128, 64], F32)
        invi, tb_invi = sb(gst, "invi", [128, 32], F32)
        rw, tb_rw = sb(gst, "rw", [128, NT, NE], F32)
        selt, tb_sel = sb(gst, "selt", [128, 2], F32)
        limf, tb_limf = sb(gst, "limf", [128, NT], F32)
        limq, tb_limq = sb(gst, "limq", [128, NT], F32)
        cx.dma("sp", selt[:], sel_in, W=[tb_sel])
        cx.dma("sp", limf[:], limf_in, W=[tb_limf])
        cx.dma("sp", limq[:], limq_in, W=[tb_limq])
        xst = contextlib.ExitStack()
        xT, tb_xT = sb(xst, "xT", [128, KC, T], BF16)

        def blend(dst, src, tq, R, W):
            sv = src.rearrange("p (j two q) -> p j two q", two=2, q=128)
            dv = dst.rearrange("p (j q) -> p j q", q=128)
            vts(dv, sv[:, :, 0, :], selt[:, 0:1], None, ALU.mult, None, R + [tb_sel], W)
            vstt(dv, sv[:, :, 1, :], selt[:, 1:2], dv, ALU.mult, ALU.add, R + [tb_sel] + W, W)
        cx.dma("sp", ident_f[:], ident_in, W=[tb_idf])
        cx.dma("sp", inva[:], inva_in, W=[tb_inva])
        cx.dma("sp", invi[:], invi_in, W=[tb_invi])
        V(lambda: nc.vector.tensor_copy(out=ident[:], in_=ident_f[:]), [tb_idf], [tb_id])

        psS, tb_psS = pp(gst, "psS", [128, 2048], F32)
        psA, tb_psA = pp(gst, "psA", [128, 512], F32)
        psB, tb_psB = pp(gst, "psB", [128, 512], F32)
        psT, tb_psT = pp(gst, "psT", [128, 1024], BF16)
        psO, tb_psO = pp(gst, "psO", [128, 512], F32)
        psAB = [(psA, tb_psA), (psB, tb_psB)]
        psQ = [(psS[:, i * 512:(i + 1) * 512], TB()) for i in range(4)]
        tb_psQ = [q[1] for q in psQ]

        def transpose_tile_to_xT(src_bf, tb_src, tt, st_tmp, xT=xT, tb_xT=tb_xT):
            for k0 in range(0, KC, 8):
                k1 = min(KC, k0 + 8)
                for k in range(k0, k1):
                    TR(psT[:, (k - k0) * 128:(k - k0 + 1) * 128], src_bf[:, k * 128:(k + 1) * 128], ident[:],
                       [tb_src, tb_id], [tb_psT])
                A(lambda: nc.scalar.copy(out=xT[:, k0:k1, tt * 128:(tt + 1) * 128],
                                         in_=psT[:, 0:(k1 - k0) * 128].rearrange("p (k t) -> p k t", t=128)),
                  [tb_psT], [tb_xT])

        def layer_norm_stage(st, src_kind, g_row, b_row, dst_final=None, router=None, ntq=None, res_blend=False,
                             xdst=None, tb_xdst=None):
            ntq = NT if ntq is None else ntq
            if xdst is None:
                xdst, tb_xdst = xT, tb_xT
            gt, tb_g = sb(st, "ln_g", [128, D], F32)
            bt, tb_b = sb(st, "ln_b", [128, D], F32)
            bcast_row(gt[:], tb_g, g_row, D)
            bcast_row(bt[:], tb_b, b_row, D)
            if router is not None:
                wr, tb_wr = sb(st, "wr", [128, NE, D], F32)
                for e in range(NE):
                    cx.dma("sp", wr[:, e, :], router[e].partition_broadcast(128), W=[tb_wr])
            nst = (D + 511) // 512
            bufs = []
            for i in range(1 if router is not None else 2):
                xa, tb_xa = sb(st, "ln_xa%d" % i, [128, D], F32)
                xb, tb_xb = sb(st, "ln_xb%d" % i, [128, D], F32)
                xh, tb_xh = sb(st, "ln_xh%d" % i, [128, D], BF16)
                stt, tb_st = sb(st, "ln_st%d" % i, [128, nst, 6], F32)
                mv, tb_mv = sb(st, "ln_mv%d" % i, [128, 8], F32)
                lg, tb_lg = sb(st, "ln_lg%d" % i, [128, 16], F32)
                bufs.append((xa, tb_xa, xb, tb_xb, xh, tb_xh, stt, tb_st, mv, tb_mv, lg, tb_lg))
            add_d, tb_add = (pre_d, tb_pre) if src_kind == "pre" else (yf_d, tb_yf)
            for tt in range(ntq):
                xa, tb_xa, xb, tb_xb, xh, tb_xh, stt, tb_st, mv, tb_mv, lg, tb_lg = bufs[tt % len(bufs)]
                rs = slice(tt * 128, (tt + 1) * 128)
                if res_blend:
                    rs1 = slice((2 * tt) * 128, (2 * tt + 1) * 128)
                    rs2 = slice((2 * tt + 1) * 128, (2 * tt + 2) * 128)
                    cx.dma("sp", xa[:], xres_d[rs1, :], R=[tb_xres[2 * tt]], W=[tb_xa])
                    cx.dma("sp", xb[:], xres_d[rs2, :], R=[tb_xres[2 * tt + 1]], W=[tb_xb])
                else:
                    cx.dma("sp", xa[:], xres_d[rs, :], R=[tb_xres[tt]], W=[tb_xa])
                if res_blend:
                    vts(xa[:], xa[:], selt[:, 0:1], None, ALU.mult, None, [tb_xa, tb_sel], [tb_xa])
                    vstt(xa[:], xb[:], selt[:, 1:2], xa[:], ALU.mult, ALU.add, [tb_xa, tb_xb, tb_sel], [tb_xa])
                cx.dma("sp", xb[:], add_d[rs, :], R=[tb_add[tt]], W=[tb_xb])
                vstt(xa[:], xa[:], ALPHA, xb[:], ALU.mult, ALU.add, [tb_xa, tb_xb], [tb_xa])
                for j in range(nst):
                    V(lambda j=j: nc.vector.bn_stats(out=stt[:, j, :], in_=xa[:, j * 512:min(D, (j + 1) * 512)]),
                      [tb_xa], [tb_st])
                V(lambda: nc.vector.bn_aggr(out=mv[:, 0:2], in_=stt[:].rearrange("p a b -> p (a b)")), [tb_st], [tb_mv])
                vts(mv[:, 2:3], mv[:, 1:2], LN_EPS, None, ALU.add, None, [tb_mv], [tb_mv])
                act(mv[:, 3:4], mv[:, 2:3], AF.Sqrt, [tb_mv], [tb_mv])
                V(lambda: nc.vector.reciprocal(out=mv[:, 4:5], in_=mv[:, 3:4]), [tb_mv], [tb_mv])
                vts(xa[:], xa[:], mv[:, 0:1], mv[:, 4:5], ALU.subtract, ALU.mult, [tb_xa, tb_mv], [tb_xa])
                vtt(xa[:], xa[:], gt[:], ALU.mult, [tb_xa, tb_g], [tb_xa])
                vtt(xa[:], xa[:], bt[:], ALU.add, [tb_xa, tb_b], [tb_xa])
                if dst_final is not None:
                    ev = cx.dma("sp", dst_final[rs, :], xa[:], R=[tb_xa])
                    cx.last_out.append(ev)
                    continue
                cx.dma("sp", xres_d[rs, :], xa[:], R=[tb_xa], W=[tb_xres[tt]])
                A(lambda: nc.scalar.copy(out=xh[:], in_=xa[:]), [tb_xa], [tb_xh])
                transpose_tile_to_xT(xh, tb_xh, tt, st, xdst, tb_xdst)
                if router is not None:
                    for e in range(NE):
                        vtt(xb[:], xa[:], wr[:, e, :], ALU.mult, [tb_xa, tb_wr], [tb_xb])
                        V(lambda e=e: nc.vector.reduce_sum(out=lg[:, e:e + 1], in_=xb[:], axis=AX.X), [tb_xb], [tb_lg])
                    V(lambda: nc.vector.max(out=lg[:, 8:16], in_=lg[:, 0:NE]), [tb_lg], [tb_lg])
                    vts(mv[:, 5:6], lg[:, 8:9], -1.0, None, ALU.mult, None, [tb_lg], [tb_mv])
                    act(rw[:, tt, :], lg[:, 0:NE], AF.Exp, [tb_lg, tb_mv], [tb_rw], bias=mv[:, 5:6], scale=1.0)
                    act(mv[:, 6:7], lg[:, 9:10], AF.Exp, [tb_lg, tb_mv], [tb_mv], bias=mv[:, 5:6], scale=1.0)
                    vts(mv[:, 6:7], mv[:, 6:7], 1.0, None, ALU.add, None, [tb_mv], [tb_mv])
                    V(lambda: nc.vector.reciprocal(out=mv[:, 7:8], in_=mv[:, 6:7]), [tb_mv], [tb_mv])
                    vts(lg[:, 0:NE], lg[:, 0:NE], lg[:, 9:10], mv[:, 7:8], ALU.is_ge, ALU.mult, [tb_lg, tb_mv], [tb_lg])
                    vtt(rw[:, tt, :], rw[:, tt, :], lg[:, 0:NE], ALU.mult, [tb_rw, tb_lg], [tb_rw])

        def load_input_stage(st, s):
            xa, tb_xa = sb(st, "li_xa", [128, D], F32)
            xh, tb_xh = sb(st, "li_xh", [128, D], BF16)
            for tt in range(NT):
                rs = slice(tt * 128, (tt + 1) * 128)
                cx.dma("sp", xa[:], x_in[s, rs, :], W=[tb_xa])
                cx.dma("sp", xres_d[rs, :], xa[:], R=[tb_xa], W=[tb_xres[tt]])
                A(lambda: nc.scalar.copy(out=xh[:], in_=xa[:]), [tb_xa], [tb_xh])
                transpose_tile_to_xT(xh, tb_xh, tt, st)

        def rope_tables(st, pos_ap, NT, inv, tb_inv, nf, name):
            cos, tb_c = sb(st, name + "_c", [128, NT, nf], F32)
            sin, tb_s = sb(st, name + "_s", [128, NT, nf], F32)
            pi_, tb_pi = sb(st, name + "_pi", [128, NT], I32)
            pf, tb_pf = sb(st, name + "_pf", [128, NT], F32)
            k_, tb_k = sb(st, name + "_k", [128, NT, nf], F32)
            halfpi, tb_hp = sb(st, name + "_hp", [128, 1], F32)
            V(lambda: nc.vector.memset(halfpi[:], float(np.pi / 2)), [], [tb_hp])
            cx.dma("sp", pi_[:], pos_ap.rearrange("(t p) -> p t", p=128), W=[tb_pi], allow_slow_non_contiguous=True)
            V(lambda: nc.vector.tensor_copy(out=pf[:], in_=pi_[:]), [tb_pi], [tb_pf])
            for tt in range(NT):
                vts(sin[:, tt, :], inv[:, 0:nf], pf[:, tt:tt + 1], None, ALU.mult, None, [tb_inv, tb_pf], [tb_s])
            sf, kf, cf = (sin[:].rearrange("p a b -> p (a b)"), k_[:].rearrange("p a b -> p (a b)"),
                          cos[:].rearrange("p a b -> p (a b)"))
            vts(kf, sf, 1.0 / TWO_PI, MAGIC, ALU.mult, ALU.add, [tb_s], [tb_k])
            vts(kf, kf, MAGIC, None, ALU.subtract, None, [tb_k], [tb_k])
            vstt(sf, kf, -C1, sf, ALU.mult, ALU.add, [tb_k, tb_s], [tb_s])
            vstt(sf, kf, -C2, sf, ALU.mult, ALU.add, [tb_k, tb_s], [tb_s])
            vts(sf, sf, float(np.pi), float(-np.pi), ALU.min, ALU.max, [tb_s], [tb_s])
            vts(kf, sf, -1.0, None, ALU.mult, None, [tb_s], [tb_k])
            vtt(kf, kf, sf, ALU.max, [tb_k, tb_s], [tb_k])
            act(cf, kf, AF.Sin, [tb_k, tb_hp], [tb_c], bias=halfpi[:, 0:1], scale=-1.0)
            act(sf, sf, AF.Sin, [tb_s], [tb_s])
            return cos, tb_c, sin, tb_s

        def mixer(l, s, split):
            W = w_in[l]
            TQ = TQL if split else T
            NTQ, NBQ = TQ // 128, TQ // 512
            lim_t, tb_lim = (limq, tb_limq) if split else (limf, tb_limf)
            with scope() as mst:
                if split:
                    xq, tb_xq = sb(mst, "xqm", [128, KC, TQ], BF16)
                    for k0 in range(KC):
                        blend(xq[:, k0, :], xT[:, k0, :], TQ, [tb_xT], [tb_xq])
                else:
                    xq, tb_xq = xT, tb_xT
                mixer_body(l, s, split, W, TQ, NTQ, NBQ, xq, tb_xq, lim_t, tb_lim)

        def mixer_body(l, s, split, W, TQ, NTQ, NBQ, xq, tb_xq, lim_t, tb_lim):
            with scope() as st:
                kT, tb_kT = sb(st, "kT", [128, G, T], BF16)
                kiT, tb_kiT = sb(st, "kiT", [128, T], BF16)
                va, tb_va = sb(st, "va", [128, NT, G, 132], BF16)
                wi, tb_wi = sb(st, "wi", [128, NT, HI], F32)
                with scope() as sa:
                    ca, tb_ca, sa_, tb_sa = rope_tables(sa, pos_in[s], NT, inva, tb_inva, 64, "ra")
                    ci, tb_ci, si_, tb_si = rope_tables(sa, pos_in[s], NT, invi, tb_invi, 32, "ri")
                    if split:
                        caq, tb_caq, saq, tb_saq = rope_tables(sa, posq_in[s], NTQ, inva, tb_inva, 64, "rqa")
                        ciq, tb_ciq, siq, tb_siq = rope_tables(sa, posq_in[s], NTQ, invi, tb_invi, 32, "rqi")
                    else:
                        caq, tb_caq, saq, tb_saq = ca, tb_ca, sa_, tb_sa
                        ciq, tb_ciq, siq, tb_siq = ci, tb_ci, si_, tb_si
                    wb = [sb(sa, "wb%d" % i, [128, KC, 512], BF16) for i in range(2)]
                    rb = [sb(sa, "rb%d" % i, [128, 512], BF16) for i in range(2)]
                    t1, tb_t1 = sb(sa, "t1", [128, 256], F32)
                    t2, tb_t2 = sb(sa, "t2", [128, 256], F32)
                    ob = [sb(sa, "ob%d" % i, [128, 4, 128], BF16) for i in range(2)]
                    V(lambda: nc.vector.memset(va[:], 1.0), [], [tb_va])
                    blocks = []
                    for h0 in range(0, H, 4):
                        blocks.append(("q", O_Q + h0 * 128, min(4, H - h0) * 128, h0))
                    for g0 in range(0, G, 4):
                        blocks.append(("k", O_K + g0 * 128, min(4, G - g0) * 128, g0))
                    for g0 in range(0, G, 4):
                        blocks.append(("v", O_V + g0 * 128, min(4, G - g0) * 128, g0))
                    for h0 in range(0, HI, 8):
                        blocks.append(("qi", O_QI + h0 * 64, min(8, HI - h0) * 64, h0))
                    blocks.append(("ki", O_KI, 64 + HI, 0))
                    blocks.append(("wi", O_KI, 64 + HI, 0))

                    def rope(dst, src, n, half, cos, tb_c, sin, tb_s, tt, tbs_src, tb_dst):
                        nh = n // (2 * half)
                        s3 = src[:, 0:n].rearrange("p (h two d) -> p h two d", two=2, d=half)
                        d3 = dst[:, 0:n].rearrange("p (h two d) -> p h two d", two=2, d=half)
                        a3 = t1[:, 0:n // 2].rearrange("p (h d) -> p h d", d=half)
                        b3 = t2[:, 0:n // 2].rearrange("p (h d) -> p h d", d=half)
                        cb = cos[:, tt, :].unsqueeze(1).broadcast_to([128, nh, half])
                        sbb = sin[:, tt, :].unsqueeze(1).broadcast_to([128, nh, half])
                        x1, x2 = s3[:, :, 0, :], s3[:, :, 1, :]
                        vtt(a3, x1, cb, ALU.mult, tbs_src + [tb_c], [tb_t1])
                        vtt(b3, x2, sbb, ALU.mult, tbs_src + [tb_s], [tb_t2])
                        vtt(d3[:, :, 0, :], a3, b3, ALU.subtract, [tb_t1, tb_t2], [tb_dst])
                        vtt(a3, x2, cb, ALU.mult, tbs_src + [tb_c], [tb_t1])
                        vtt(b3, x1, sbb, ALU.mult, tbs_src + [tb_s], [tb_t2])
                        vtt(d3[:, :, 1, :], a3, b3, ALU.add, [tb_t1, tb_t2], [tb_dst])

                    cnt = 0
                    for bi, (kind, c0, n, i0) in enumerate(blocks):
                        wt, tb_w = wb[bi % 2]
                        wload(wt, tb_w, W[:, c0:c0 + n], D, n)
                        own = kind in ("q", "qi", "wi")
                        xs, tb_xs = (xq, tb_xq) if own else (xT, tb_xT)
                        for tt in range(NTQ if own else NT):
                            ps, tb_ps = psAB[cnt % 2]
                            r_, tb_r = rb[cnt % 2]
                            o_, tb_o = ob[cnt % 2]
                            cnt += 1
                            for k in range(KC):
                                MM(ps[:, 0:n], xs[:, k, tt * 128:(tt + 1) * 128], wt[:, k, 0:n], k == 0, k == KC - 1,
                                   [tb_xs, tb_w], [tb_ps])
                            ts = slice(tt * 128, (tt + 1) * 128)
                            if kind == "q":
                                rope(r_, ps, n, 64, caq, tb_caq, saq, tb_saq, tt, [tb_ps], tb_r)
                            if kind == "k":
                                rope(r_, ps, n, 64, ca, tb_ca, sa_, tb_sa, tt, [tb_ps], tb_r)
                            if kind in ("q", "k"):
                                nh = n // 128
                                for j in range(nh):
                                    TR(psT[:, j * 128:(j + 1) * 128], r_[:, j * 128:(j + 1) * 128], ident[:],
                                       [tb_r, tb_id], [tb_psT])
                                src = psT[:, 0:n].rearrange("p (h t) -> p h t", t=128)
                                if kind == "k":
                                    A(lambda: nc.scalar.copy(out=kT[:, i0:i0 + nh, ts], in_=src), [tb_psT], [tb_kT])
                                else:
                                    A(lambda: nc.scalar.copy(out=o_[:, 0:nh, :], in_=src), [tb_psT], [tb_o])
                                    cx.dma("sp", qT_d[tt, :, i0:i0 + nh, :], o_[:, 0:nh, :], R=[tb_o], W=[tb_qT[tt]])
                            elif kind == "v":
                                ng = n // 128
                                A(lambda: nc.scalar.copy(out=va[:, tt, i0:i0 + ng, 0:128],
                                                         in_=ps[:, 0:n].rearrange("p (g d) -> p g d", d=128)),
                                  [tb_ps], [tb_va])
                            elif kind == "qi":
                                rope(r_, ps, n, 32, ciq, tb_ciq, siq, tb_siq, tt, [tb_ps], tb_r)
                                npair = n // 128
                                for j in range(npair):
                                    TR(psT[:, j * 128:(j + 1) * 128], r_[:, j * 128:(j + 1) * 128], ident[:],
                                       [tb_r, tb_id], [tb_psT])
                                A(lambda: nc.scalar.copy(out=o_[:, 0:npair, :],
                                                         in_=psT[:, 0:npair * 128].rearrange("p (h t) -> p h t", t=128)),
                                  [tb_psT], [tb_o])
                                cx.dma("sp", qiT_d[tt, :, i0 // 2:i0 // 2 + npair, :], o_[:, 0:npair, :], R=[tb_o],
                                       W=[tb_qiT[tt]])
                            elif kind == "wi":
                                V(lambda: nc.vector.tensor_copy(out=wi[:, tt, :], in_=ps[:, 64:64 + HI]), [tb_ps], [tb_wi])
                            else:
                                rope(r_, ps, 64, 32, ci, tb_ci, si_, tb_si, tt, [tb_ps], tb_r)
                                V(lambda: nc.vector.tensor_copy(out=r_[:, 64:128], in_=r_[:, 0:64]), [tb_r], [tb_r])
                                TR(psT[:, 0:128], r_[:, 0:128], ident[:], [tb_r, tb_id], [tb_psT])
                                A(lambda: nc.scalar.copy(out=kiT[:, ts], in_=psT[:, 0:128]), [tb_psT], [tb_kiT])

                with scope() as sbk:
                    iota_s, tb_io = sb(sbk, "iota_s", [128, T], F32)
                    iota_p, tb_ip = sb(sbk, "iota_p", [128, 1], F32)
                    P(lambda: nc.gpsimd.iota(iota_s[:], pattern=[[1, T]], base=0, channel_multiplier=0,
                                             allow_small_or_imprecise_dtypes=True), [], [tb_io])
                    qt = [sb(sbk, "qt%d" % i, [128, H, 128], BF16) for i in range(2)]
                    qit = [sb(sbk, "qit%d" % i, [128, HI // 2, 128], BF16) for i in range(2)]
                    acc, tb_acc = sb(sbk, "acc", [128, T], F32)
                    wk, tb_wk = sb(sbk, "wk", [128, T], F32)
                    msk, tb_msk = sb(sbk, "msk", [128, T], BF16)
                    rl = [sb(sbk, "rl%d" % i, [128, 512], F32) for i in range(2)]
                    m8, tb_m8 = sb(sbk, "m8", [128, 8], F32)
                    sm, tb_sm = sb(sbk, "sm", [128, 8], F32)
                    ee = [sb(sbk, "ee%d" % i, [128, T], BF16) for i in range(2)]
                    pt = [sb(sbk, "pt%d" % i, [128, NT, 128], BF16) for i in range(2)]
                    ot, tb_ot = sb(sbk, "ot", [128, H, 128], BF16)
                    yo, tb_yo = sb(sbk, "yo", [128, H, 128], BF16)
                    scale = 128.0 ** -0.5
                    for tt in range(NTQ):
                        q_, tb_q = qt[tt % 2]
                        qi_, tb_qi = qit[tt % 2]
                        cx.dma("sp", q_[:], qT_d[tt], R=[tb_qT[tt]], W=[tb_q])
                        cx.dma("sp", qi_[:], qiT_d[tt], R=[tb_qiT[tt]], W=[tb_qi])
                        NKT = min(NT, (2 * tt + 2) if split else (tt + 1))
                        KW = NKT * 128
                        NKB = (KW + 511) // 512
                        vts(acc[:, 0:KW], iota_s[:, 0:KW], lim_t[:, tt:tt + 1], NEG1, ALU.is_ge, ALU.mult,
                            [tb_io, tb_lim], [tb_acc])
                        c2 = 0
                        for h in range(HI):
                            hp, ho = h // 2, (h % 2) * 64
                            for kb in range(NKB):
                                ps, tb_ps = psAB[c2 % 2]
                                r_, tb_r = rl[c2 % 2]
                                c2 += 1
                                k0_, k1_ = kb * 512, min(KW, (kb + 1) * 512)
                                w_ = k1_ - k0_
                                MM(ps[:, 0:w_], qi_[ho:ho + 64, hp, :], kiT[ho:ho + 64, k0_:k1_], True, True,
                                   [tb_qi, tb_kiT], [tb_ps])
                                act(r_[:, 0:w_], ps[:, 0:w_], AF.Relu, [tb_ps], [tb_r])
                                vstt(acc[:, k0_:k1_], r_[:, 0:w_], wi[:, tt, h:h + 1],
                                     acc[:, k0_:k1_], ALU.mult, ALU.add, [tb_r, tb_wi, tb_acc], [tb_acc])
                        if KW > TOPK:
                            cur, tb_cur = acc, tb_acc
                            for r in range(NR):
                                V(lambda cur=cur: nc.vector.max(out=m8[:], in_=cur[:, 0:KW]), [tb_cur], [tb_m8])
                                if r < NR - 1:
                                    V(lambda cur=cur: nc.vector.match_replace(out=wk[:, 0:KW], in_to_replace=m8[:],
                                                                              in_values=cur[:, 0:KW], imm_value=NEG2),
                                      [tb_cur, tb_m8], [tb_wk])
                                    cur, tb_cur = wk, tb_wk
                            vts(sm[:, 0:1], m8[:, 7:8], NEG1 * 0.1, None, ALU.max, None, [tb_m8], [tb_sm])
                            vts(msk[:, 0:KW], acc[:, 0:KW], sm[:, 0:1], None, ALU.is_ge, None, [tb_acc, tb_sm], [tb_msk])
                        else:
                            vts(msk[:, 0:KW], acc[:, 0:KW], NEG1 * 0.1, None, ALU.is_ge, None, [tb_acc], [tb_msk])
                        if DBG == "attn0" and tt == 0:
                            dbg = {}
                            for nm, shp, dt in (("dbg_acc", [128, T], F32), ("dbg_msk", [128, T], BF16), ("dbg_e", [128, T], BF16),
                                                ("dbg_sm", [128, 8], F32), ("dbg_m8", [128, 8], F32), ("dbg_ot", [128, H * 128], BF16),
                                                ("dbg_pso", [128, 132], F32), ("dbg_va", [128, NT * G * 132], BF16),
                                                ("dbg_kT", [128, G * T], BF16), ("dbg_kiT", [128, T], BF16), ("dbg_wi", [128, NT * HI], F32)):
                                dbg[nm] = nc.dram_tensor(nm, shp, dt, kind="ExternalOutput").ap()
                            evs = [cx.dma("sp", dbg["dbg_acc"], acc[:], R=[tb_acc]), cx.dma("sp", dbg["dbg_msk"], msk[:], R=[tb_msk]),
                                   cx.dma("sp", dbg["dbg_m8"], m8[:], R=[tb_m8]),
                                   cx.dma("sp", dbg["dbg_va"], va[:].rearrange("p a b c -> p (a b c)"), R=[tb_va]),
                                   cx.dma("sp", dbg["dbg_kT"], kT[:].rearrange("p a b -> p (a b)"), R=[tb_kT]),
                                   cx.dma("sp", dbg["dbg_kiT"], kiT[:], R=[tb_kiT]),
                                   cx.dma("sp", dbg["dbg_wi"], wi[:].rearrange("p a b -> p (a b)"), R=[tb_wi])]
                        for h in range(H):
                            g = h // HPG
                            e_, tb_e = ee[h % 2]
                            p_, tb_p = pt[h % 2]
                            for kb in range(NKB):
                                k0_, k1_ = kb * 512, min(KW, (kb + 1) * 512)
                                MM(psS[:, k0_:k1_], q_[:, h, :], kT[:, g, k0_:k1_], True, True,
                                   [tb_q, tb_kT], [tb_psQ[kb]])
                            V(lambda: nc.vector.reduce_max(out=sm[:, 1:2], in_=psS[:, 0:KW], axis=AX.X), tb_psQ[:NKB], [tb_sm])
                            vts(sm[:, 2:3], sm[:, 1:2], -scale, None, ALU.mult, None, [tb_sm], [tb_sm])
                            act(e_[:, 0:KW], psS[:, 0:KW], AF.Exp, tb_psQ[:NKB] + [tb_sm], [tb_e], bias=sm[:, 2:3], scale=scale)
                            vtt(e_[:, 0:KW], e_[:, 0:KW], msk[:, 0:KW], ALU.mult, [tb_e, tb_msk], [tb_e])
                            for k0 in range(0, NKT, 8):
                                k1 = min(NKT, k0 + 8)
                                for k in range(k0, k1):
                                    TR(psT[:, (k - k0) * 128:(k - k0 + 1) * 128], e_[:, k * 128:(k + 1) * 128], ident[:],
                                       [tb_e, tb_id], [tb_psT])
                                A(lambda k0=k0, k1=k1: nc.scalar.copy(
                                    out=p_[:, k0:k1, :], in_=psT[:, 0:(k1 - k0) * 128].rearrange("p (k t) -> p k t", t=128)),
                                  [tb_psT], [tb_p])
                            for k in range(NKT):
                                MM(psO[:, 0:129], p_[:, k, :], va[:, k, g, 0:129], k == 0, k == NKT - 1,
                                   [tb_p, tb_va], [tb_psO])
                            V(lambda: nc.vector.reciprocal(out=sm[:, 3:4], in_=psO[:, 128:129]), [tb_psO], [tb_sm])
                            if DBG == "attn0" and tt == 0 and h == 0:
                                V(lambda: nc.vector.tensor_copy(out=wk[:, 0:132], in_=psO[:, 0:132]), [tb_psO], [tb_wk])
                                evs += [cx.dma("sp", dbg["dbg_e"], e_[:], R=[tb_e]), cx.dma("sp", dbg["dbg_sm"], sm[:], R=[tb_sm]),
                                        cx.dma("sp", dbg["dbg_pso"], wk[:, 0:132], R=[tb_wk])]
                            vts(ot[:, h, :], psO[:, 0:128], sm[:, 3:4], None, ALU.mult, None, [tb_psO, tb_sm], [tb_ot])
                        if DBG == "attn0" and tt == 0:
                            evs.append(cx.dma("sp", dbg["dbg_ot"], ot[:].rearrange("p a b -> p (a b)"), R=[tb_ot]))
                            for ev in evs:
                                cx._wait("sp", ev)
                            stopf[0] = True
                            break
                        for h0 in range(0, H, 8):
                            h1 = min(H, h0 + 8)
                            for h in range(h0, h1):
                                TR(psT[:, (h - h0) * 128:(h - h0 + 1) * 128], ot[:, h, :], ident[:], [tb_ot, tb_id], [tb_psT])
                            A(lambda h0=h0, h1=h1: nc.scalar.copy(
                                out=yo[:, h0:h1, :], in_=psT[:, 0:(h1 - h0) * 128].rearrange("p (k t) -> p k t", t=128)),
                              [tb_psT], [tb_yo])
                        cx.dma("sp", yT_d[0:NAC, :, tt * 128:(tt + 1) * 128].rearrange("h p t -> p h t"), yo[:],
                               R=[tb_yo], W=tb_yT[0:NAC])

            if DBG == "attnB":
                stopf[0] = True
            if stopf[0]:
                return
            with scope() as st:
                wc = [sb(st, "wc%d" % i, [128, KC, 128], BF16) for i in range(3)]
                wcn = [0]

                def proj_fm(c0, dst, tb_dst, func=None, own=False):
                    wt, tb_w = wc[wcn[0] % 3]
                    wcn[0] += 1
                    wload(wt, tb_w, W[:, c0:c0 + 128], D, 128)
                    xs, tb_xs = (xq, tb_xq) if own else (xT, tb_xT)
                    for tb_ in range(NBQ if own else NB):
                        ps, tb_ps = psQ[tb_]
                        for k in range(KC):
                            MM(ps, wt[:, k, :], xs[:, k, tb_ * 512:(tb_ + 1) * 512], k == 0, k == KC - 1,
                               [tb_w, tb_xs], [tb_ps])
                        if func is None:
                            A(lambda tb_=tb_, ps=ps: nc.scalar.copy(out=dst[:, tb_ * 512:(tb_ + 1) * 512], in_=ps),
                              [tb_ps], [tb_dst])
                        else:
                            act(dst[:, tb_ * 512:(tb_ + 1) * 512], ps, func, [tb_ps], [tb_dst])

                f = [sb(st, "f%d" % i, [128, T], F32) for i in range(7)]
                hb = [sb(st, "hb%d" % i, [128, T], BF16) for i in range(4)]
                yb, tb_yb = sb(st, "yb", [128, T], BF16)
                col, tb_col = sb(st, "col", [128, 16], F32)
                cw, tb_cw = sb(st, "cw", [128, 4, 128], BF16)
                wa_t, tb_wa = sb(st, "wa_t", [128, 1, 128], BF16)
                wx_t, tb_wx = sb(st, "wx_t", [128, 1, 128], BF16)

                def colvec(j, src_row, c0):
                    cx.dma("sp", col[:, j:j + 1], src_row[c0:c0 + 128].rearrange("(p o) -> p o", o=1), W=[tb_col],
                           allow_slow_non_contiguous=True)

                for g in range(4):
                    win = (2, 4, 8, 16)[g]
                    ncg = PW // 128
                    for i in range(ncg):
                        c = g * ncg + i
                        (p_, tb_p), (a_, tb_a), (b_, tb_b) = f[0], f[1], f[2]
                        proj_fm(O_P + c * 128, p_, tb_p)
                        src, tb_src = p_, tb_p
                        sh = 1
                        flip = 0
                        while sh < win:
                            dst, tb_dst = (a_, tb_a) if flip == 0 else (b_, tb_b)
                            flip ^= 1
                            V(lambda dst=dst, src=src, sh=sh: nc.vector.tensor_copy(out=dst[:, 0:sh], in_=src[:, 0:sh]),
                              [tb_src], [tb_dst])
                            vtt(dst[:, sh:T], src[:, sh:T], src[:, 0:T - sh], ALU.add, [tb_src], [tb_dst])
                            src, tb_src = dst, tb_dst
                            sh *= 2
                        d_, tb_d = (a_, tb_a) if src is b_ else (b_, tb_b)
                        vstt(d_[:, win - 1:T], src[:, win - 1:T], 1.0 / win, p_[:, win - 1:T], ALU.mult, ALU.subtract,
                             [tb_src, tb_p], [tb_d])
                        for t in range(win - 1):
                            vstt(d_[:, t:t + 1], src[:, t:t + 1], 1.0 / (t + 1), p_[:, t:t + 1], ALU.mult, ALU.subtract,
                                 [tb_src, tb_p], [tb_d])
                        if split:
                            blend(hb[i][0][:, 0:TQ], d_[:], TQ, [tb_d], [hb[i][1]])
                        else:
                            V(lambda d_=d_, i=i: nc.vector.tensor_copy(out=hb[i][0][:], in_=d_[:]), [tb_d], [hb[i][1]])
                    for j in range(ncg):
                        c = g * ncg + j
                        wload(cw, tb_cw, pool_w[l, g][:, j * 128:(j + 1) * 128], PW, 128)
                        colvec(0, pool_scale[l], c * 128)
                        gt_, tb_gt = f[3]
                        proj_fm(O_GT + (NAC + c) * 128, gt_, tb_gt, AF.Sigmoid, own=True)
                        for tb_ in range(NBQ):
                            ps, tb_ps = psAB[tb_ % 2]
                            for i in range(ncg):
                                MM(ps[:, :], cw[:, i, :], hb[i][0][:, tb_ * 512:(tb_ + 1) * 512], i == 0, i == ncg - 1,
                                   [tb_cw, hb[i][1]], [tb_ps])
                            vstt(yb[:, tb_ * 512:(tb_ + 1) * 512], ps[:, :], col[:, 0:1], gt_[:, tb_ * 512:(tb_ + 1) * 512],
                                 ALU.mult, ALU.mult, [tb_ps, tb_col, tb_gt], [tb_yb])
                        cx.dma("sp", yT_d[NAC + c][:, 0:TQ], yb[:, 0:TQ], R=[tb_yb], W=[tb_yT[NAC + c]])
                for n in range(KC):
                    (xr, tb_xr), (xc, tb_xc), (r_, tb_r), (i_, tb_i), (a_, tb_a), (g_, tb_g), (gt_, tb_gt) = f
                    xcb, tb_xcb = hb[0]
                    proj_fm(O_XR + n * 128, xr, tb_xr)
                    for tap in range(4):
                        colvec(tap, conv_w[l, tap], n * 128)
                    colvec(4, conv_b[l], n * 128)
                    colvec(5, lru_ba[l], n * 128)
                    colvec(6, lru_bx[l], n * 128)
                    colvec(7, lru_lam[l], n * 128)
                    act(col[:, 8:9], col[:, 7:8], AF.Exp, [tb_col], [tb_col], scale=-1.0)
                    act(col[:, 9:10], col[:, 8:9], AF.Ln, [tb_col], [tb_col], bias=1.0, scale=1.0)
                    vts(col[:, 10:11], col[:, 9:10], -8.0, None, ALU.mult, None, [tb_col], [tb_col])
                    vts(xc[:], xr[:], col[:, 3:4], col[:, 4:5], ALU.mult, ALU.add, [tb_xr, tb_col], [tb_xc])
                    for tap in range(3):
                        sh = 3 - tap
                        vstt(xc[:, sh:T], xr[:, 0:T - sh], col[:, tap:tap + 1], xc[:, sh:T], ALU.mult, ALU.add,
                             [tb_xr, tb_col, tb_xc], [tb_xc])
                    V(lambda: nc.vector.tensor_copy(out=xcb[:], in_=xc[:]), [tb_xc], [tb_xcb])
                    wload(wa_t, tb_wa, lru_wa[l, n], 128, 128)
                    wload(wx_t, tb_wx, lru_wx[l, n], 128, 128)
                    for tb_ in range(NB):
                        tsl = slice(tb_ * 512, (tb_ + 1) * 512)
                        psa, tb_psa = psAB[0]
                        psb, tb_psb = psAB[1]
                        MM(psa[:, :], wa_t[:, 0, :], xcb[:, tsl], True, True, [tb_wa, tb_xcb], [tb_psa])
                        MM(psb[:, :], wx_t[:, 0, :], xcb[:, tsl], True, True, [tb_wx, tb_xcb], [tb_psb])
                        act(r_[:, tsl], psa[:, :], AF.Sigmoid, [tb_psa, tb_col], [tb_r], bias=col[:, 5:6], scale=1.0)
                        act(i_[:, tsl], psb[:, :], AF.Sigmoid, [tb_psb, tb_col], [tb_i], bias=col[:, 6:7], scale=1.0)
                    act(a_[:], r_[:], AF.Exp, [tb_r, tb_col], [tb_a], scale=col[:, 10:11])
                    vtt(r_[:], a_[:], a_[:], ALU.mult, [tb_a], [tb_r])
                    vts(r_[:], r_[:], -1.0, 1.0, ALU.mult, ALU.add, [tb_r], [tb_r])
                    vts(r_[:], r_[:], 0.0, None, ALU.max, None, [tb_r], [tb_r])
                    act(r_[:], r_[:], AF.Sqrt, [tb_r], [tb_r])
                    vtt(i_[:], i_[:], xc[:], ALU.mult, [tb_i, tb_xc], [tb_i])
                    vtt(i_[:], i_[:], r_[:], ALU.mult, [tb_i, tb_r], [tb_i])
                    V(lambda: nc.vector.tensor_tensor_scan(out=xc[:], data0=a_[:], data1=i_[:], initial=0.0,
                                                           op0=ALU.mult, op1=ALU.add), [tb_a, tb_i], [tb_xc])
                    if split:
                        blend(a_[:, 0:TQ], xc[:], TQ, [tb_xc], [tb_a])
                        hq, tb_hq = a_, tb_a
                    else:
                        hq, tb_hq = xc, tb_xc
                    q_ = slice(0, TQ)
                    proj_fm(O_GR + n * 128, g_, tb_g, own=True)
                    vtt(r_[:, q_], g_[:, q_], g_[:, q_], ALU.mult, [tb_g], [tb_r])
                    vts(r_[:, q_], r_[:, q_], 0.044715, 1.0, ALU.mult, ALU.add, [tb_r], [tb_r])
                    vtt(r_[:, q_], r_[:, q_], g_[:, q_], ALU.mult, [tb_r, tb_g], [tb_r])
                    act(r_[:, q_], r_[:, q_], AF.Sigmoid, [tb_r], [tb_r], scale=float(2.0 * np.sqrt(2.0 / np.pi)))
                    vtt(r_[:, q_], r_[:, q_], g_[:, q_], ALU.mult, [tb_r, tb_g], [tb_r])
                    vtt(r_[:, q_], r_[:, q_], hq[:, q_], ALU.mult, [tb_r, tb_hq], [tb_r])
                    proj_fm(O_GT + (NAC + KC + n) * 128, gt_, tb_gt, AF.Sigmoid, own=True)
                    vtt(yb[:, q_], r_[:, q_], gt_[:, q_], ALU.mult, [tb_r, tb_gt], [tb_yb])
                    cx.dma("sp", yT_d[NAC + KC + n][:, q_], yb[:, q_], R=[tb_yb], W=[tb_yT[NAC + KC + n]])
                for c in range(NAC):
                    gt_, tb_gt = f[3]
                    ya, tb_ya = hb[c % 2]
                    proj_fm(O_GT + c * 128, gt_, tb_gt, AF.Sigmoid, own=True)
                    cx.dma("sp", ya[:, 0:TQ], yT_d[c][:, 0:TQ], R=[tb_yT[c]], W=[tb_ya])
                    vtt(yb[:, 0:TQ], ya[:, 0:TQ], gt_[:, 0:TQ], ALU.mult, [tb_ya, tb_gt], [tb_yb])
                    cx.dma("sp", yT_d[c][:, 0:TQ], yb[:, 0:TQ], R=[tb_yb], W=[tb_yT[c]])
                for c in range(NMC):
                    ya, tb_ya = hb[c % 4]
                    cx.dma("sp", ya[:, 0:TQ], yT_d[c][:, 0:TQ], R=[tb_yT[c]], W=[tb_ya])
                    cx.dma("sp", mT_d[0:NTQ, :, c, :].rearrange("t p q -> p t q"),
                           ya[:, 0:TQ].rearrange("p (t q) -> p t q", q=128), R=[tb_ya], W=[tb_mT])

            with scope() as st:
                HK = NMC // 2
                wo = [sb(st, "wo%d" % i, [128, HK, 512], BF16) for i in range(2)]
                mt = [sb(st, "mt%d" % i, [128, NMC, 128], BF16) for i in range(2)]
                po = [sb(st, "po%d" % i, [128, 512], F32) for i in range(2)]
                ditems = [(nb, h) for nb in range(D // 512) for h in range(2)]

                def dload(i):
                    nb, h = ditems[i]
                    wload(wo[h][0], wo[h][1], w_out[l][h * HK * 128:(h + 1) * HK * 128, nb * 512:(nb + 1) * 512], HK * 128, 512)

                dload(0)
                cd = 0
                for i, (nb, h) in enumerate(ditems):
                    if i + 1 < len(ditems):
                        dload(i + 1)
                    w_, tb_w = wo[h]
                    for tt in range(NTQ):
                        m_, tb_m = mt[cd % 2]
                        o_, tb_o = po[cd % 2]
                        ps, tb_ps = psAB[cd % 2]
                        cd += 1
                        cx.dma("sp", m_[:], mT_d[tt], R=[tb_mT], W=[tb_m])
                        for c in range(HK):
                            MM(ps[:, :], m_[:, h * HK + c, :], w_[:, c, :], c == 0, c == HK - 1, [tb_m, tb_w], [tb_ps])
                        A(lambda o_=o_, ps=ps: nc.scalar.copy(out=o_[:], in_=ps[:, :]), [tb_ps], [tb_o])
                        dst = pre_d[tt * 128:(tt + 1) * 128, nb * 512:(nb + 1) * 512]
                        if h == 0:
                            cx.dma("sp", dst, o_[:], R=[tb_o], W=[tb_pre[tt]])
                        else:
                            cx.dma("pool", dst, o_[:], R=[tb_o], W=[tb_pre[tt]], accum_op=ALU.add)

        def ffn(wsets, F, xq, tb_xq, TQ, GF):
            nfc = (F + 127) // 128
            assert F % 128 == 0 and GF % 4 == 0
            NTQ, NBQ = TQ // 128, TQ // 512
            with scope() as st:
                wgc = [sb(st, "wgc%d" % i, [128, KC, 512], BF16) for i in range(2)]
                wuc = [sb(st, "wuc%d" % i, [128, KC, 512], BF16) for i in range(2)]
                hT, tb_hT = sb(st, "hT", [128, GF, TQ], BF16)
                wdb = [sb(st, "wdb%d" % i, [128, GF, 512], BF16) for i in range(3)]
                sg = [sb(st, "sg%d" % i, [128, 512], F32) for i in range(2)]
                po = [sb(st, "fpo%d" % i, [128, 512], F32) for i in range(2)]
                c3 = [0]
                items = []
                hcnt = 0
                dcnt = 0
                for wi_, (wg, wu, wd, scale_e) in enumerate(wsets):
                    for f0 in range(0, nfc, GF):
                        f1 = min(nfc, f0 + GF)
                        for fb in range(f0, f1, 4):
                            items.append(("H", f0, f1, fb, hcnt, wi_))
                            hcnt += 1
                        for nb in range(D // 512):
                            items.append(("D", f0, f1, nb, dcnt, wi_))
                            dcnt += 1

                def load(it):
                    kind, f0, f1, j, hc, wi_ = it
                    wg, wu, wd, scale_e = wsets[wi_]
                    if kind == "H":
                        n = (min(f1, j + 4) - j) * 128
                        g_, tb_g = wgc[hc % 2]
                        u_, tb_u = wuc[hc % 2]
                        wload(g_, tb_g, wg[:, j * 128:j * 128 + n], D, n)
                        wload(u_, tb_u, wu[:, j * 128:j * 128 + n], D, n)
                    else:
                        w_, tb_w = wdb[hc % 3]
                        wload(w_, tb_w, wd[f0 * 128:f1 * 128, j * 512:(j + 1) * 512], (f1 - f0) * 128, 512)

                def compute(it):
                    kind, f0, f1, j, hc, wi_ = it
                    wg, wu, wd, scale_e = wsets[wi_]
                    first = wi_ == 0
                    if kind == "H":
                        g_, tb_g = wgc[hc % 2]
                        u_, tb_u = wuc[hc % 2]
                        for fi in range(j, min(f1, j + 4)):
                            cs = slice((fi - j) * 128, (fi - j + 1) * 128)
                            for tb_ in range(NBQ):
                                tsl = slice(tb_ * 512, (tb_ + 1) * 512)
                                psa, tb_psa = psQ[(2 * tb_) % 4]
                                psb, tb_psb = psQ[(2 * tb_ + 1) % 4]
                                s_, tb_s = sg[tb_ % 2]
                                for k in range(KC):
                                    MM(psa, g_[:, k, cs], xq[:, k, tsl], k == 0, k == KC - 1, [tb_g, tb_xq], [tb_psa])
                                for k in range(KC):
                                    MM(psb, u_[:, k, cs], xq[:, k, tsl], k == 0, k == KC - 1, [tb_u, tb_xq], [tb_psb])
                                act(s_[:, :], psa, AF.Silu, [tb_psa], [tb_s])
                                vtt(hT[:, fi - f0, tsl], s_[:, :], psb, ALU.mult, [tb_s, tb_psb], [tb_hT])
                    else:
                        nb = j
                        w_, tb_w = wdb[hc % 3]
                        for tt in range(NTQ):
                            ps, tb_ps = psAB[c3[0] % 2]
                            o_, tb_o = po[c3[0] % 2]
                            c3[0] += 1
                            for fi in range(f0, f1):
                                MM(ps[:, :], hT[:, fi - f0, tt * 128:(tt + 1) * 128], w_[:, fi - f0, :], fi == f0,
                                   fi == f1 - 1, [tb_hT, tb_w], [tb_ps])
                            if scale_e is None:
                                A(lambda o_=o_, ps=ps: nc.scalar.copy(out=o_[:], in_=ps[:, :]), [tb_ps], [tb_o])
                            else:
                                vts(o_[:], ps[:, :], rw[:, tt, scale_e:scale_e + 1], None, ALU.mult, None,
                                    [tb_ps, tb_rw], [tb_o])
                            dst = yf_d[tt * 128:(tt + 1) * 128, nb * 512:(nb + 1) * 512]
                            if first and f0 == 0:
                                cx.dma("pool", dst, o_[:], R=[tb_o], W=[tb_yf[tt]])
                            else:
                                cx.dma("pool", dst, o_[:], R=[tb_o], W=[tb_yf[tt]], accum_op=ALU.add)

                pos = {}
                for ii, it in enumerate(items):
                    pos[(it[0], it[4])] = ii
                nl = 0
                for ii, it in enumerate(items):
                    while nl < len(items) and nl <= ii + 2:
                        kind, hc = items[nl][0], items[nl][4]
                        prev = pos.get((kind, hc - (2 if kind == "H" else 3)))
                        if prev is not None and prev >= ii:
                            break
                        load(items[nl])
                        nl += 1
                    compute(it)
                assert nl == len(items)


        def drain_all():
            for tbl in (tb_xres, tb_pre, tb_yf, tb_yT, tb_qT, tb_qiT, [tb_mT]):
                for b in tbl:
                    for ev in list(b.w.values()):
                        cx._wait("sp", ev)

        for s in range(NSEQ):
            with scope() as st:
                load_input_stage(st, s)
            for l in range(L):
                split = SPLIT > 1 and l == L - 1
                ntq_l = (TQL // 128) if split else NT
                mixer(l, s, split)
                if stopf[0]:
                    drain_all()
                    return nc
                if DBG == "mixer%d" % l:
                    drain_all()
                    return nc
                with contextlib.ExitStack() as tl:
                    if split:
                        cx.barrier()
                        xst.close()
                        xs_, tb_xs_ = sb(tl, "xq2", [128, KC, TQL], BF16)
                        tq_l, gf_l = TQL, 16
                    else:
                        xs_, tb_xs_ = xT, tb_xT
                        tq_l, gf_l = T, 8
                    with scope() as st:
                        layer_norm_stage(st, "pre", ln_mix_g[l], ln_mix_b[l],
                                         router=(routerT[l // 2] if l % 2 == 1 else None), ntq=ntq_l, res_blend=split,
                                         xdst=xs_, tb_xdst=tb_xs_)
                    if l % 2 == 0:
                        ffn([(dw_gate[l // 2], dw_up[l // 2], dw_down[l // 2], None)], DFF, xs_, tb_xs_, tq_l, gf_l)
                    else:
                        ffn([(mw_gate[l // 2, e], mw_up[l // 2, e], mw_down[l // 2, e], e) for e in range(NE)], DEXP,
                            xs_, tb_xs_, tq_l, gf_l)
                    with scope() as st:
                        layer_norm_stage(st, "yf", ln_ffn_g[l], ln_ffn_b[l],
                                         dst_final=(y_out[s] if l == L - 1 else None), ntq=ntq_l)
                    cx.barrier()
        for ev in cx.last_out:
            cx._wait("sp", ev)
        xst.close()
    return nc


def consts():
    ident = np.eye(128, dtype=np.float32)
    inva = np.tile((10000.0 ** (-np.arange(0, 128, 2, dtype=np.float32) / 128.0)).astype(np.float32)[None, :], (128, 1))
    invi = np.tile((10000.0 ** (-np.arange(0, 64, 2, dtype=np.float32) / 64.0)).astype(np.float32)[None, :], (128, 1))
    return ident, inva, invi


def run(cfg, inputs):
    ncores = cfg["NCORES"]
    split = cfg.get("SPLIT", 1)
    nseq = cfg["B"] * split // ncores
    T = cfg["T"]
    tq = T // split
    nt = T // 128
    nc = build_nc(cfg)
    ident, inva, invi = consts()
    rows = np.arange(128)[:, None] + 128 * np.arange(nt)[None, :]
    limf = ((rows // 64 + 1) * 64).astype(np.float32)
    in_maps = []
    for c in range(ncores):
        b0, hf = (c // split) * nseq, c % split
        m = {}
        for k, v in inputs.items():
            v = np.asarray(v)
            if k in ("x", "positions"):
                m[k] = np.ascontiguousarray(v[b0:b0 + nseq])
            elif k == "moe_router":
                m["moe_routerT"] = np.ascontiguousarray(np.swapaxes(v, 1, 2))
            else:
                m[k] = v
        pos = np.asarray(inputs["positions"])[b0:b0 + nseq]
        if split == 1:
            m["positions_q"] = np.ascontiguousarray(pos)
        else:
            m["positions_q"] = np.ascontiguousarray(pos.reshape(nseq, nt // split, split, 128)[:, :, hf, :].reshape(nseq, tq))
        sel = np.zeros((128, 2), np.float32)
        sel[:, hf] = 1.0
        m["c_sel"] = sel
        m["c_limf"] = limf
        gl = (np.arange(nt)[None, :] * split + hf) * 128 + np.arange(128)[:, None]
        m["c_limq"] = ((gl // 64 + 1) * 64).astype(np.float32)
        m["c_ident"], m["c_inva"], m["c_invi"] = ident, inva, invi
        in_maps.append(m)
    if cfg.get("ONLY_MAPS"):
        return nc, in_maps
    if cfg.get("SUBSET") is not None:
        sub = cfg["SUBSET"]
        r = run_bass_kernel_spmd(nc, [in_maps[c] for c in sub], core_ids=list(range(len(sub))))
        return r.results
    res = run_bass_kernel_spmd(nc, in_maps, core_ids=list(range(ncores)))
    if cfg.get("STOP"):
        return res.results
    out = np.empty((cfg["B"], T, cfg["D"]), np.float32)
    for c in range(ncores):
        b0, hf = (c // split) * nseq, c % split
        if split == 1:
            out[b0:b0 + nseq] = res.results[c]["y"]
        else:
            out[b0].reshape(nt // split, split, 128, cfg["D"])[:, hf] = res.results[c]["y"][0].reshape(nt // split, 128, cfg["D"])
    return out


def kernel(**inputs):
    return run(FULL, inputs).astype(np.float32)
```

```python
import contextlib
import numpy as np
import ml_dtypes
import concourse.bass as bass
import concourse.mybir as mybir
from concourse.bass_utils import run_bass_kernel_spmd

F32 = mybir.dt.float32
BF16 = mybir.dt.bfloat16
I32 = mybir.dt.int32
AF = mybir.ActivationFunctionType
ALU = mybir.AluOpType
AX = mybir.AxisListType

FULL = dict(D=2048, T=2048, B=4, L=2, H=16, G=4, HI=16, TOPK=256, DFF=5504, NE=8, DEXP=7168, NCORES=8, SPLIT=2)
LN_EPS = 1e-5
LIM_SEM = 24000
NEG1 = -1.0e30
NEG2 = -3.0e38
MAGIC = 12582912.0
TWO_PI = 6.283185307179586
C1 = 6.28125
C2 = TWO_PI - 6.28125


class TB:
    __slots__ = ("w", "weng", "r")

    def __init__(self):
        self.w = {}
        self.weng = None
        self.r = {}


class Ctx:
    def __init__(self, nc, st):
        self.nc = nc
        self.st = st
        self.nsem = 0
        self.LIM = LIM_SEM
        self.eng = {"pe": nc.tensor, "act": nc.scalar, "dve": nc.vector, "pool": nc.gpsimd, "sp": nc.sync}
        self.sem = {}
        self.cnt = {}
        self.seen = {k: {} for k in self.eng}
        for k in ("pe", "act", "dve", "pool"):
            self.sem[k] = st.enter_context(nc.semaphore("s_" + k))
            self.cnt[k] = 0
        self.NS = 8
        self.dsem = {}
        self.di = {}
        for q in ("sp", "pool", "act"):
            self.dsem[q] = [st.enter_context(nc.semaphore("d_%s%d" % (q, i))) for i in range(self.NS)]
            self.di[q] = 0
        self.semid = {}
        self.last_out = []

    def _wait(self, e, ev):
        if ev is None:
            return
        sem, val = ev
        k = id(sem)
        if self.seen[e].get(k, 0) >= val:
            return
        self.eng[e].wait_ge(sem, val)
        self.seen[e][k] = val

    def _deps(self, e, R, W, acc_ok=False):
        for b in R:
            for ev in b.w.values():
                self._wait(e, ev)
        for b in W:
            for k, ev in b.w.items():
                if not (acc_ok and k == "pe"):
                    self._wait(e, ev)
            for ev in b.r.values():
                self._wait(e, ev)

    def _mark(self, key, e, ev, R, W):
        for b in R:
            b.r[key] = ev
        for b in W:
            b.w[key] = ev
            b.r = {}

    def op(self, e, fn, R=(), W=(), acc_ok=False):
        self._deps(e, R, W, acc_ok)
        ins = fn()
        self.cnt[e] += 1
        ins.then_inc(self.sem[e], 1)
        ev = (self.sem[e], self.cnt[e])
        self._mark(e, e, ev, R, W)
        return ev

    def barrier(self):
        evs = [(self.sem[k], self.cnt[k]) for k in ("pe", "act", "dve", "pool") if self.cnt[k] > 0]
        for q in self.dsem:
            di = self.di[q]
            for j in range(self.NS):
                if di > j:
                    evs.append((self.dsem[q][j], 16 * ((di - j - 1) // self.NS + 1)))
        for e in ("pe", "act", "dve", "pool", "sp"):
            for ev in evs:
                self._wait(e, ev)
        for k in ("pe", "act", "dve", "pool"):
            if self.cnt[k] > self.LIM:
                self.nsem += 1
                self.sem[k] = self.st.enter_context(self.nc.semaphore("s_%s_%d" % (k, self.nsem)))
                self.cnt[k] = 0
        for q in self.dsem:
            if 16 * (self.di[q] // self.NS + 1) > self.LIM:
                self.nsem += 1
                self.dsem[q] = [self.st.enter_context(self.nc.semaphore("d_%s%d_%d" % (q, i, self.nsem)))
                                for i in range(self.NS)]
                self.di[q] = 0

    def dma(self, q, out, in_, R=(), W=(), **kw):
        self._deps(q, R, W)
        i = self.di[q]
        sem = self.dsem[q][i % self.NS]
        if i >= self.NS:
            self._wait(q, (sem, 16 * (i // self.NS)))
        ins = self.eng[q].dma_start(out=out, in_=in_, **kw)
        ins.then_inc(sem, 16)
        ev = (sem, 16 * (i // self.NS + 1))
        self.di[q] = i + 1
        self._mark("dma_%s_%d" % (q, i % self.NS), "dma", ev, R, W)
        return ev


def build_nc(cfg):
    D, T, L, H, G, HI = cfg["D"], cfg["T"], cfg["L"], cfg["H"], cfg["G"], cfg["HI"]
    TOPK, DFF, NE, DEXP = cfg["TOPK"], cfg["DFF"], cfg["NE"], cfg["DEXP"]
    SPLIT = cfg.get("SPLIT", 1)
    NSEQ = cfg["B"] * SPLIT // cfg["NCORES"]
    assert SPLIT == 1 or NSEQ == 1
    KC, NT, NB = D // 128, T // 128, T // 512
    TQL = T // SPLIT
    HPG = H // G
    AW, KW, IW = H * 128, G * 128, HI * 64
    MIXW = AW + 2 * D
    O_Q, O_K, O_V = 0, AW, AW + KW
    O_QI = AW + 2 * KW
    O_KI = O_QI + IW
    O_WI = O_KI + 64
    O_P = O_WI + HI
    O_XR = O_P + D
    O_GR = O_XR + D
    O_GT = O_GR + D
    INC = O_GT + MIXW
    NMC = MIXW // 128
    NAC = AW // 128
    ALPHA = (2 * L) ** 0.25
    NR = TOPK // 8
    PW = D // 4

    nc = bass.Bass("TRN2", target_bir_lowering=False)

    def din(name, shape, dt=F32):
        return nc.dram_tensor(name, list(shape), dt, kind="ExternalInput").ap()

    x_in = din("x", [NSEQ, T, D])
    pos_in = din("positions", [NSEQ, T], I32)
    posq_in = din("positions_q", [NSEQ, TQL], I32)
    sel_in = din("c_sel", [128, 2])
    limf_in = din("c_limf", [128, NT])
    limq_in = din("c_limq", [128, NT])
    w_in = din("mix_w_in", [L, D, INC])
    w_out = din("mix_w_out", [L, MIXW, D])
    pool_w = din("pool_w", [L, 4, PW, PW])
    pool_scale = din("pool_scale", [L, D])
    conv_w = din("conv_w", [L, 4, D])
    conv_b = din("conv_b", [L, D])
    lru_wa = din("lru_wa", [L, KC, 128, 128])
    lru_ba = din("lru_ba", [L, D])
    lru_wx = din("lru_wx", [L, KC, 128, 128])
    lru_bx = din("lru_bx", [L, D])
    lru_lam = din("lru_lam", [L, D])
    ln_mix_g = din("ln_mix_g", [L, D])
    ln_mix_b = din("ln_mix_b", [L, D])
    ln_ffn_g = din("ln_ffn_g", [L, D])
    ln_ffn_b = din("ln_ffn_b", [L, D])
    dw_gate = din("dense_w_gate", [(L + 1) // 2, D, DFF])
    dw_up = din("dense_w_up", [(L + 1) // 2, D, DFF])
    dw_down = din("dense_w_down", [(L + 1) // 2, DFF, D])
    routerT = din("moe_routerT", [max(L // 2, 1), NE, D])
    mw_gate = din("moe_w_gate", [max(L // 2, 1), NE, D, DEXP])
    mw_up = din("moe_w_up", [max(L // 2, 1), NE, D, DEXP])
    mw_down = din("moe_w_down", [max(L // 2, 1), NE, DEXP, D])
    ident_in = din("c_ident", [128, 128])
    inva_in = din("c_inva", [128, 64])
    invi_in = din("c_invi", [128, 32])
    y_out = nc.dram_tensor("y", [NSEQ, TQL, D], F32, kind="ExternalOutput").ap()

    DBG = cfg.get("STOP")

    def dscr(name, shape, dt):
        if DBG and name in ("xres_d", "pre_d", "yf_d", "yT_d", "qT_d", "qiT_d"):
            return nc.dram_tensor(name, list(shape), dt, kind="ExternalOutput").ap()
        return nc.dram_tensor(name, list(shape), dt, kind="Internal").ap()

    xres_d = dscr("xres_d", [T, D], F32)
    pre_d = dscr("pre_d", [T, D], F32)
    yf_d = dscr("yf_d", [T, D], F32)
    yT_d = dscr("yT_d", [NMC, 128, T], BF16)
    mT_d = dscr("mT_d", [NT, 128, NMC, 128], BF16)
    qT_d = dscr("qT_d", [NT, 128, H, 128], BF16)
    qiT_d = dscr("qiT_d", [NT, 128, HI // 2, 128], BF16)
    tb_xres = [TB() for _ in range(NT)]
    tb_pre = [TB() for _ in range(NT)]
    tb_yf = [TB() for _ in range(NT)]
    tb_yT = [TB() for _ in range(NMC)]
    tb_mT = TB()
    tb_qT = [TB() for _ in range(NT)]
    tb_qiT = [TB() for _ in range(NT)]

    class _Stop(Exception):
        pass

    with contextlib.ExitStack() as gst:
        cx = Ctx(nc, gst)
        stopf = [False]

        @contextlib.contextmanager
        def scope():
            with contextlib.ExitStack() as st_:
                yield st_
                cx.barrier()

        uid = [0]

        def sb(st, name, shape, dt):
            uid[0] += 1
            return st.enter_context(nc.sbuf_tensor("%s_%d" % (name, uid[0]), list(shape), dt)), TB()

        def pp(st, name, shape, dt):
            return st.enter_context(nc.psum_tensor(name, list(shape), dt)), TB()

        V = lambda fn, R=(), W=(): cx.op("dve", fn, R, W)
        A = lambda fn, R=(), W=(): cx.op("act", fn, R, W)
        P = lambda fn, R=(), W=(): cx.op("pool", fn, R, W)

        def MM(out, lhsT, rhs, start, stop, R, W):
            return cx.op("pe", lambda: nc.tensor.matmul(out, lhsT, rhs, start=start, stop=stop), R, W, acc_ok=not start)

        def TR(out, in_, ident, R, W):
            return cx.op("pe", lambda: nc.tensor.transpose(out, in_, ident), R, W, acc_ok=True)

        def act(out, in_, func, R, W, **kw):
            return A(lambda: nc.scalar.activation(out=out, in_=in_, func=func, **kw), R, W)

        def vts(out, in0, s1, s2, op0, op1, R, W):
            if op1 is None:
                return V(lambda: nc.vector.tensor_scalar(out=out, in0=in0, scalar1=s1, scalar2=None, op0=op0), R, W)
            return V(lambda: nc.vector.tensor_scalar(out=out, in0=in0, scalar1=s1, scalar2=s2, op0=op0, op1=op1), R, W)

        def vtt(out, in0, in1, op, R, W):
            return V(lambda: nc.vector.tensor_tensor(out=out, in0=in0, in1=in1, op=op), R, W)

        def vstt(out, in0, sc, in1, op0, op1, R, W):
            return V(lambda: nc.vector.scalar_tensor_tensor(out=out, in0=in0, scalar=sc, in1=in1, op0=op0, op1=op1), R, W)

        def wload(dst, dtb, src2d, rows, cols):
            nk = rows // 128
            for k0 in range(0, nk, 4):
                k1 = min(nk, k0 + 4)
                cx.dma("pool", dst[:, k0:k1, 0:cols],
                       src2d[k0 * 128:k1 * 128, :].rearrange("(k p) n -> p k n", p=128), W=[dtb])

        def bcast_row(dst, dtb, src_row, n):
            cx.dma("sp", dst, src_row.partition_broadcast(128), W=[dtb])

        ident_f, tb_idf = sb(gst, "ident_f", [128, 128], F32)
        ident, tb_id = sb(gst, "ident", [128, 128], BF16)
        inva, tb_inva = sb(gst, "inva", [128, 64], F32)
        invi, tb_invi = sb(gst, "invi", [128, 32], F32)
        rw, tb_rw = sb(gst, "rw", [128, NT, NE], F32)
        selt, tb_sel = sb(gst, "selt", [128, 2], F32)
        limf, tb_limf = sb(gst, "limf", [128, NT], F32)
        limq, tb_limq = sb(gst, "limq", [128, NT], F32)
        cx.dma("sp", selt[:], sel_in, W=[tb_sel])
        cx.dma("sp", limf[:], limf_in, W=[tb_limf])
        cx.dma("sp", limq[:], limq_in, W=[tb_limq])
        xst = contextlib.ExitStack()
        xT, tb_xT = sb(xst, "xT", [128, KC, T], BF16)

        def blend(dst, src, tq, R, W):
            sv = src.rearrange("p (j two q) -> p j two q", two=2, q=128)
            dv = dst.rearrange("p (j q) -> p j q", q=128)
            vts(dv, sv[:, :, 0, :], selt[:, 0:1], None, ALU.mult, None, R + [tb_sel], W)
            vstt(dv, sv[:, :, 1, :], selt[:, 1:2], dv, ALU.mult, ALU.add, R + [tb_sel] + W, W)
        cx.dma("sp", ident_f[:], ident_in, W=[tb_idf])
        cx.dma("sp", inva[:], inva_in, W=[tb_inva])
        cx.dma("sp", invi[:], invi_in, W=[tb_invi])
        V(lambda: nc.vector.tensor_copy(out=ident[:], in_=ident_f[:]), [tb_idf], [tb_id])

        psS, tb_psS = pp(gst, "psS", [128, 2048], F32)
        psA, tb_psA = pp(gst, "psA", [128, 512], F32)
        psB, tb_psB = pp(gst, "psB", [128, 512], F32)
        psT, tb_psT = pp(gst, "psT", [128, 1024], BF16)
        psO, tb_psO = pp(gst, "psO", [128, 512], F32)
        psAB = [(psA, tb_psA), (psB, tb_psB)]
        psQ = [(psS[:, i * 512:(i + 1) * 512], TB()) for i in range(4)]
        tb_psQ = [q[1] for q in psQ]

        def transpose_tile_to_xT(src_bf, tb_src, tt, st_tmp, xT=xT, tb_xT=tb_xT):
            for k0 in range(0, KC, 8):
                k1 = min(KC, k0 + 8)
                for k in range(k0, k1):
                    TR(psT[:, (k - k0) * 128:(k - k0 + 1) * 128], src_bf[:, k * 128:(k + 1) * 128], ident[:],
                       [tb_src, tb_id], [tb_psT])
                A(lambda: nc.scalar.copy(out=xT[:, k0:k1, tt * 128:(tt + 1) * 128],
                                         in_=psT[:, 0:(k1 - k0) * 128].rearrange("p (k t) -> p k t", t=128)),
                  [tb_psT], [tb_xT])

        def layer_norm_stage(st, src_kind, g_row, b_row, dst_final=None, router=None, ntq=None, res_blend=False,
                             xdst=None, tb_xdst=None, add_sb=None, tb_add_sb=None):
            ntq = NT if ntq is None else ntq
            if xdst is None:
                xdst, tb_xdst = xT, tb_xT
            gt, tb_g = sb(st, "ln_g", [128, D], F32)
            bt, tb_b = sb(st, "ln_b", [128, D], F32)
            bcast_row(gt[:], tb_g, g_row, D)
            bcast_row(bt[:], tb_b, b_row, D)
            if router is not None:
                wr, tb_wr = sb(st, "wr", [128, NE, D], F32)
                for e in range(NE):
                    cx.dma("sp", wr[:, e, :], router[e].partition_broadcast(128), W=[tb_wr])
            nst = (D + 511) // 512
            bufs = []
            for i in range(1 if router is not None else 2):
                xa, tb_xa = sb(st, "ln_xa%d" % i, [128, D], F32)
                xb, tb_xb = sb(st, "ln_xb%d" % i, [128, D], F32)
                xh, tb_xh = sb(st, "ln_xh%d" % i, [128, D], BF16)
                stt, tb_st = sb(st, "ln_st%d" % i, [128, nst, 6], F32)
                mv, tb_mv = sb(st, "ln_mv%d" % i, [128, 8], F32)
                lg, tb_lg = sb(st, "ln_lg%d" % i, [128, 16], F32)
                bufs.append((xa, tb_xa, xb, tb_xb, xh, tb_xh, stt, tb_st, mv, tb_mv, lg, tb_lg))
            add_d, tb_add = (pre_d, tb_pre) if src_kind == "pre" else (yf_d, tb_yf)
            for tt in range(ntq):
                xa, tb_xa, xb, tb_xb, xh, tb_xh, stt, tb_st, mv, tb_mv, lg, tb_lg = bufs[tt % len(bufs)]
                rs = slice(tt * 128, (tt + 1) * 128)
                if res_blend:
                    rs1 = slice((2 * tt) * 128, (2 * tt + 1) * 128)
                    rs2 = slice((2 * tt + 1) * 128, (2 * tt + 2) * 128)
                    cx.dma("sp", xa[:], xres_d[rs1, :], R=[tb_xres[2 * tt]], W=[tb_xa])
                    cx.dma("sp", xb[:], xres_d[rs2, :], R=[tb_xres[2 * tt + 1]], W=[tb_xb])
                else:
                    cx.dma("sp", xa[:], xres_d[rs, :], R=[tb_xres[tt]], W=[tb_xa])
                if res_blend:
                    vts(xa[:], xa[:], selt[:, 0:1], None, ALU.mult, None, [tb_xa, tb_sel], [tb_xa])
                    vstt(xa[:], xb[:], selt[:, 1:2], xa[:], ALU.mult, ALU.add, [tb_xa, tb_xb, tb_sel], [tb_xa])
                if add_sb is not None:
                    vstt(xa[:], xa[:], ALPHA, add_sb[:, tt, :], ALU.mult, ALU.add, [tb_xa, tb_add_sb[tt]], [tb_xa])
                else:
                    cx.dma("sp", xb[:], add_d[rs, :], R=[tb_add[tt]], W=[tb_xb])
                    vstt(xa[:], xa[:], ALPHA, xb[:], ALU.mult, ALU.add, [tb_xa, tb_xb], [tb_xa])
                for j in range(nst):
                    V(lambda j=j: nc.vector.bn_stats(out=stt[:, j, :], in_=xa[:, j * 512:min(D, (j + 1) * 512)]),
                      [tb_xa], [tb_st])
                V(lambda: nc.vector.bn_aggr(out=mv[:, 0:2], in_=stt[:].rearrange("p a b -> p (a b)")), [tb_st], [tb_mv])
                vts(mv[:, 2:3], mv[:, 1:2], LN_EPS, None, ALU.add, None, [tb_mv], [tb_mv])
                act(mv[:, 3:4], mv[:, 2:3], AF.Sqrt, [tb_mv], [tb_mv])
                V(lambda: nc.vector.reciprocal(out=mv[:, 4:5], in_=mv[:, 3:4]), [tb_mv], [tb_mv])
                vts(xa[:], xa[:], mv[:, 0:1], mv[:, 4:5], ALU.subtract, ALU.mult, [tb_xa, tb_mv], [tb_xa])
                vtt(xa[:], xa[:], gt[:], ALU.mult, [tb_xa, tb_g], [tb_xa])
                vtt(xa[:], xa[:], bt[:], ALU.add, [tb_xa, tb_b], [tb_xa])
                if dst_final is not None:
                    ev = cx.dma("sp", dst_final[rs, :], xa[:], R=[tb_xa])
                    cx.last_out.append(ev)
                    continue
                cx.dma("sp", xres_d[rs, :], xa[:], R=[tb_xa], W=[tb_xres[tt]])
                A(lambda: nc.scalar.copy(out=xh[:], in_=xa[:]), [tb_xa], [tb_xh])
                transpose_tile_to_xT(xh, tb_xh, tt, st, xdst, tb_xdst)
                if router is not None:
                    for e in range(NE):
                        vtt(xb[:], xa[:], wr[:, e, :], ALU.mult, [tb_xa, tb_wr], [tb_xb])
                        V(lambda e=e: nc.vector.reduce_sum(out=lg[:, e:e + 1], in_=xb[:], axis=AX.X), [tb_xb], [tb_lg])
                    V(lambda: nc.vector.max(out=lg[:, 8:16], in_=lg[:, 0:NE]), [tb_lg], [tb_lg])
                    vts(mv[:, 5:6], lg[:, 8:9], -1.0, None, ALU.mult, None, [tb_lg], [tb_mv])
                    act(rw[:, tt, :], lg[:, 0:NE], AF.Exp, [tb_lg, tb_mv], [tb_rw], bias=mv[:, 5:6], scale=1.0)
                    act(mv[:, 6:7], lg[:, 9:10], AF.Exp, [tb_lg, tb_mv], [tb_mv], bias=mv[:, 5:6], scale=1.0)
                    vts(mv[:, 6:7], mv[:, 6:7], 1.0, None, ALU.add, None, [tb_mv], [tb_mv])
                    V(lambda: nc.vector.reciprocal(out=mv[:, 7:8], in_=mv[:, 6:7]), [tb_mv], [tb_mv])
                    vts(lg[:, 0:NE], lg[:, 0:NE], lg[:, 9:10], mv[:, 7:8], ALU.is_ge, ALU.mult, [tb_lg, tb_mv], [tb_lg])
                    vtt(rw[:, tt, :], rw[:, tt, :], lg[:, 0:NE], ALU.mult, [tb_rw, tb_lg], [tb_rw])

        def load_input_stage(st, s):
            xa, tb_xa = sb(st, "li_xa", [128, D], F32)
            xh, tb_xh = sb(st, "li_xh", [128, D], BF16)
            for tt in range(NT):
                rs = slice(tt * 128, (tt + 1) * 128)
                cx.dma("sp", xa[:], x_in[s, rs, :], W=[tb_xa])
                cx.dma("sp", xres_d[rs, :], xa[:], R=[tb_xa], W=[tb_xres[tt]])
                A(lambda: nc.scalar.copy(out=xh[:], in_=xa[:]), [tb_xa], [tb_xh])
                transpose_tile_to_xT(xh, tb_xh, tt, st)

        def rope_tables(st, pos_ap, NT, inv, tb_inv, nf, name):
            cos, tb_c = sb(st, name + "_c", [128, NT, nf], F32)
            sin, tb_s = sb(st, name + "_s", [128, NT, nf], F32)
            pi_, tb_pi = sb(st, name + "_pi", [128, NT], I32)
            pf, tb_pf = sb(st, name + "_pf", [128, NT], F32)
            k_, tb_k = sb(st, name + "_k", [128, NT, nf], F32)
            halfpi, tb_hp = sb(st, name + "_hp", [128, 1], F32)
            V(lambda: nc.vector.memset(halfpi[:], float(np.pi / 2)), [], [tb_hp])
            cx.dma("sp", pi_[:], pos_ap.rearrange("(t p) -> p t", p=128), W=[tb_pi], allow_slow_non_contiguous=True)
            V(lambda: nc.vector.tensor_copy(out=pf[:], in_=pi_[:]), [tb_pi], [tb_pf])
            for tt in range(NT):
                vts(sin[:, tt, :], inv[:, 0:nf], pf[:, tt:tt + 1], None, ALU.mult, None, [tb_inv, tb_pf], [tb_s])
            sf, kf, cf = (sin[:].rearrange("p a b -> p (a b)"), k_[:].rearrange("p a b -> p (a b)"),
                          cos[:].rearrange("p a b -> p (a b)"))
            vts(kf, sf, 1.0 / TWO_PI, MAGIC, ALU.mult, ALU.add, [tb_s], [tb_k])
            vts(kf, kf, MAGIC, None, ALU.subtract, None, [tb_k], [tb_k])
            vstt(sf, kf, -C1, sf, ALU.mult, ALU.add, [tb_k, tb_s], [tb_s])
            vstt(sf, kf, -C2, sf, ALU.mult, ALU.add, [tb_k, tb_s], [tb_s])
            vts(sf, sf, float(np.pi), float(-np.pi), ALU.min, ALU.max, [tb_s], [tb_s])
            vts(kf, sf, -1.0, None, ALU.mult, None, [tb_s], [tb_k])
            vtt(kf, kf, sf, ALU.max, [tb_k, tb_s], [tb_k])
            act(cf, kf, AF.Sin, [tb_k, tb_hp], [tb_c], bias=halfpi[:, 0:1], scale=-1.0)
            act(sf, sf, AF.Sin, [tb_s], [tb_s])
            return cos, tb_c, sin, tb_s

        def mixer(l, s, split):
            W = w_in[l]
            TQ = TQL if split else T
            NTQ, NBQ = TQ // 128, TQ // 512
            lim_t, tb_lim = (limq, tb_limq) if split else (limf, tb_limf)
            with scope() as mst:
                if split:
                    xq, tb_xq = sb(mst, "xqm", [128, KC, TQ], BF16)
                    for k0 in range(KC):
                        blend(xq[:, k0, :], xT[:, k0, :], TQ, [tb_xT], [tb_xq])
                else:
                    xq, tb_xq = xT, tb_xT
                mixer_body(l, s, split, W, TQ, NTQ, NBQ, xq, tb_xq, lim_t, tb_lim)

        def mixer_body(l, s, split, W, TQ, NTQ, NBQ, xq, tb_xq, lim_t, tb_lim):
            with scope() as st:
                kT, tb_kT = sb(st, "kT", [128, G, T], BF16)
                kiT, tb_kiT = sb(st, "kiT", [128, T], BF16)
                va, tb_va = sb(st, "va", [128, NT, G, 132], BF16)
                wi, tb_wi = sb(st, "wi", [128, NT, HI], F32)
                with scope() as sa:
                    ca, tb_ca, sa_, tb_sa = rope_tables(sa, pos_in[s], NT, inva, tb_inva, 64, "ra")
                    ci, tb_ci, si_, tb_si = rope_tables(sa, pos_in[s], NT, invi, tb_invi, 32, "ri")
                    if split:
                        caq, tb_caq, saq, tb_saq = rope_tables(sa, posq_in[s], NTQ, inva, tb_inva, 64, "rqa")
                        ciq, tb_ciq, siq, tb_siq = rope_tables(sa, posq_in[s], NTQ, invi, tb_invi, 32, "rqi")
                    else:
                        caq, tb_caq, saq, tb_saq = ca, tb_ca, sa_, tb_sa
                        ciq, tb_ciq, siq, tb_siq = ci, tb_ci, si_, tb_si
                    wb = [sb(sa, "wb%d" % i, [128, KC, 512], BF16) for i in range(2)]
                    rb = [sb(sa, "rb%d" % i, [128, 512], BF16) for i in range(2)]
                    t1, tb_t1 = sb(sa, "t1", [128, 256], F32)
                    t2, tb_t2 = sb(sa, "t2", [128, 256], F32)
                    ob = [sb(sa, "ob%d" % i, [128, 4, 128], BF16) for i in range(2)]
                    V(lambda: nc.vector.memset(va[:], 1.0), [], [tb_va])
                    blocks = []
                    for h0 in range(0, H, 4):
                        blocks.append(("q", O_Q + h0 * 128, min(4, H - h0) * 128, h0))
                    for g0 in range(0, G, 4):
                        blocks.append(("k", O_K + g0 * 128, min(4, G - g0) * 128, g0))
                    for g0 in range(0, G, 4):
                        blocks.append(("v", O_V + g0 * 128, min(4, G - g0) * 128, g0))
                    for h0 in range(0, HI, 8):
                        blocks.append(("qi", O_QI + h0 * 64, min(8, HI - h0) * 64, h0))
                    blocks.append(("ki", O_KI, 64 + HI, 0))
                    blocks.append(("wi", O_KI, 64 + HI, 0))

                    def rope(dst, src, n, half, cos, tb_c, sin, tb_s, tt, tbs_src, tb_dst):
                        nh = n // (2 * half)
                        s3 = src[:, 0:n].rearrange("p (h two d) -> p h two d", two=2, d=half)
                        d3 = dst[:, 0:n].rearrange("p (h two d) -> p h two d", two=2, d=half)
                        a3 = t1[:, 0:n // 2].rearrange("p (h d) -> p h d", d=half)
                        b3 = t2[:, 0:n // 2].rearrange("p (h d) -> p h d", d=half)
                        cb = cos[:, tt, :].unsqueeze(1).broadcast_to([128, nh, half])
                        sbb = sin[:, tt, :].unsqueeze(1).broadcast_to([128, nh, half])
                        x1, x2 = s3[:, :, 0, :], s3[:, :, 1, :]
                        vtt(a3, x1, cb, ALU.mult, tbs_src + [tb_c], [tb_t1])
                        vtt(b3, x2, sbb, ALU.mult, tbs_src + [tb_s], [tb_t2])
                        vtt(d3[:, :, 0, :], a3, b3, ALU.subtract, [tb_t1, tb_t2], [tb_dst])
                        vtt(a3, x2, cb, ALU.mult, tbs_src + [tb_c], [tb_t1])
                        vtt(b3, x1, sbb, ALU.mult, tbs_src + [tb_s], [tb_t2])
                        vtt(d3[:, :, 1, :], a3, b3, ALU.add, [tb_t1, tb_t2], [tb_dst])

                    cnt = 0
                    for bi, (kind, c0, n, i0) in enumerate(blocks):
                        wt, tb_w = wb[bi % 2]
                        wload(wt, tb_w, W[:, c0:c0 + n], D, n)
                        own = kind in ("q", "qi", "wi")
                        xs, tb_xs = (xq, tb_xq) if own else (xT, tb_xT)
                        for tt in range(NTQ if own else NT):
                            ps, tb_ps = psAB[cnt % 2]
                            r_, tb_r = rb[cnt % 2]
                            o_, tb_o = ob[cnt % 2]
                            cnt += 1
                            for k in range(KC):
                                MM(ps[:, 0:n], xs[:, k, tt * 128:(tt + 1) * 128], wt[:, k, 0:n], k == 0, k == KC - 1,
                                   [tb_xs, tb_w], [tb_ps])
                            ts = slice(tt * 128, (tt + 1) * 128)
                            if kind == "q":
                                rope(r_, ps, n, 64, caq, tb_caq, saq, tb_saq, tt, [tb_ps], tb_r)
                            if kind == "k":
                                rope(r_, ps, n, 64, ca, tb_ca, sa_, tb_sa, tt, [tb_ps], tb_r)
                            if kind in ("q", "k"):
                                nh = n // 128
                                for j in range(nh):
                                    TR(psT[:, j * 128:(j + 1) * 128], r_[:, j * 128:(j + 1) * 128], ident[:],
                                       [tb_r, tb_id], [tb_psT])
                                src = psT[:, 0:n].rearrange("p (h t) -> p h t", t=128)
                                if kind == "k":
                                    A(lambda: nc.scalar.copy(out=kT[:, i0:i0 + nh, ts], in_=src), [tb_psT], [tb_kT])
                                else:
                                    A(lambda: nc.scalar.copy(out=o_[:, 0:nh, :], in_=src), [tb_psT], [tb_o])
                                    cx.dma("sp", qT_d[tt, :, i0:i0 + nh, :], o_[:, 0:nh, :], R=[tb_o], W=[tb_qT[tt]])
                            elif kind == "v":
                                ng = n // 128
                                A(lambda: nc.scalar.copy(out=va[:, tt, i0:i0 + ng, 0:128],
                                                         in_=ps[:, 0:n].rearrange("p (g d) -> p g d", d=128)),
                                  [tb_ps], [tb_va])
                            elif kind == "qi":
                                rope(r_, ps, n, 32, ciq, tb_ciq, siq, tb_siq, tt, [tb_ps], tb_r)
                                npair = n // 128
                                for j in range(npair):
                                    TR(psT[:, j * 128:(j + 1) * 128], r_[:, j * 128:(j + 1) * 128], ident[:],
                                       [tb_r, tb_id], [tb_psT])
                                A(lambda: nc.scalar.copy(out=o_[:, 0:npair, :],
                                                         in_=psT[:, 0:npair * 128].rearrange("p (h t) -> p h t", t=128)),
                                  [tb_psT], [tb_o])
                                cx.dma("sp", qiT_d[tt, :, i0 // 2:i0 // 2 + npair, :], o_[:, 0:npair, :], R=[tb_o],
                                       W=[tb_qiT[tt]])
                            elif kind == "wi":
                                V(lambda: nc.vector.tensor_copy(out=wi[:, tt, :], in_=ps[:, 64:64 + HI]), [tb_ps], [tb_wi])
                            else:
                                rope(r_, ps, 64, 32, ci, tb_ci, si_, tb_si, tt, [tb_ps], tb_r)
                                V(lambda: nc.vector.tensor_copy(out=r_[:, 64:128], in_=r_[:, 0:64]), [tb_r], [tb_r])
                                TR(psT[:, 0:128], r_[:, 0:128], ident[:], [tb_r, tb_id], [tb_psT])
                                A(lambda: nc.scalar.copy(out=kiT[:, ts], in_=psT[:, 0:128]), [tb_psT], [tb_kiT])

                with scope() as sbk:
                    iota_s, tb_io = sb(sbk, "iota_s", [128, T], F32)
                    iota_p, tb_ip = sb(sbk, "iota_p", [128, 1], F32)
                    P(lambda: nc.gpsimd.iota(iota_s[:], pattern=[[1, T]], base=0, channel_multiplier=0,
                                             allow_small_or_imprecise_dtypes=True), [], [tb_io])
                    qt = [sb(sbk, "qt%d" % i, [128, H, 128], BF16) for i in range(2)]
                    qit = [sb(sbk, "qit%d" % i, [128, HI // 2, 128], BF16) for i in range(2)]
                    acc, tb_acc = sb(sbk, "acc", [128, T], F32)
                    wk, tb_wk = sb(sbk, "wk", [128, T], F32)
                    msk, tb_msk = sb(sbk, "msk", [128, T], BF16)
                    rl = [sb(sbk, "rl%d" % i, [128, 512], F32) for i in range(2)]
                    m8, tb_m8 = sb(sbk, "m8", [128, 8], F32)
                    sm, tb_sm = sb(sbk, "sm", [128, 8], F32)
                    ee = [sb(sbk, "ee%d" % i, [128, T], BF16) for i in range(2)]
                    pt = [sb(sbk, "pt%d" % i, [128, NT, 128], BF16) for i in range(2)]
                    ot, tb_ot = sb(sbk, "ot", [128, H, 128], BF16)
                    yo, tb_yo = sb(sbk, "yo", [128, H, 128], BF16)
                    scale = 128.0 ** -0.5
                    for tt in range(NTQ):
                        q_, tb_q = qt[tt % 2]
                        qi_, tb_qi = qit[tt % 2]
                        cx.dma("sp", q_[:], qT_d[tt], R=[tb_qT[tt]], W=[tb_q])
                        cx.dma("sp", qi_[:], qiT_d[tt], R=[tb_qiT[tt]], W=[tb_qi])
                        NKT = min(NT, (2 * tt + 2) if split else (tt + 1))
                        KW = NKT * 128
                        NKB = (KW + 511) // 512
                        vts(acc[:, 0:KW], iota_s[:, 0:KW], lim_t[:, tt:tt + 1], NEG1, ALU.is_ge, ALU.mult,
                            [tb_io, tb_lim], [tb_acc])
                        c2 = 0
                        for h in range(HI):
                            hp, ho = h // 2, (h % 2) * 64
                            for kb in range(NKB):
                                ps, tb_ps = psAB[c2 % 2]
                                r_, tb_r = rl[c2 % 2]
                                c2 += 1
                                k0_, k1_ = kb * 512, min(KW, (kb + 1) * 512)
                                w_ = k1_ - k0_
                                MM(ps[:, 0:w_], qi_[ho:ho + 64, hp, :], kiT[ho:ho + 64, k0_:k1_], True, True,
                                   [tb_qi, tb_kiT], [tb_ps])
                                act(r_[:, 0:w_], ps[:, 0:w_], AF.Relu, [tb_ps], [tb_r])
                                vstt(acc[:, k0_:k1_], r_[:, 0:w_], wi[:, tt, h:h + 1],
                                     acc[:, k0_:k1_], ALU.mult, ALU.add, [tb_r, tb_wi, tb_acc], [tb_acc])
                        if KW > TOPK:
                            cur, tb_cur = acc, tb_acc
                            for r in range(NR):
                                V(lambda cur=cur: nc.vector.max(out=m8[:], in_=cur[:, 0:KW]), [tb_cur], [tb_m8])
                                if r < NR - 1:
                                    V(lambda cur=cur: nc.vector.match_replace(out=wk[:, 0:KW], in_to_replace=m8[:],
                                                                              in_values=cur[:, 0:KW], imm_value=NEG2),
                                      [tb_cur, tb_m8], [tb_wk])
                                    cur, tb_cur = wk, tb_wk
                            vts(sm[:, 0:1], m8[:, 7:8], NEG1 * 0.1, None, ALU.max, None, [tb_m8], [tb_sm])
                            vts(msk[:, 0:KW], acc[:, 0:KW], sm[:, 0:1], None, ALU.is_ge, None, [tb_acc, tb_sm], [tb_msk])
                        else:
                            vts(msk[:, 0:KW], acc[:, 0:KW], NEG1 * 0.1, None, ALU.is_ge, None, [tb_acc], [tb_msk])
                        if DBG == "attn0" and tt == 0:
                            dbg = {}
                            for nm, shp, dt in (("dbg_acc", [128, T], F32), ("dbg_msk", [128, T], BF16), ("dbg_e", [128, T], BF16),
                                                ("dbg_sm", [128, 8], F32), ("dbg_m8", [128, 8], F32), ("dbg_ot", [128, H * 128], BF16),
                                                ("dbg_pso", [128, 132], F32), ("dbg_va", [128, NT * G * 132], BF16),
                                                ("dbg_kT", [128, G * T], BF16), ("dbg_kiT", [128, T], BF16), ("dbg_wi", [128, NT * HI], F32)):
                                dbg[nm] = nc.dram_tensor(nm, shp, dt, kind="ExternalOutput").ap()
                            evs = [cx.dma("sp", dbg["dbg_acc"], acc[:], R=[tb_acc]), cx.dma("sp", dbg["dbg_msk"], msk[:], R=[tb_msk]),
                                   cx.dma("sp", dbg["dbg_m8"], m8[:], R=[tb_m8]),
                                   cx.dma("sp", dbg["dbg_va"], va[:].rearrange("p a b c -> p (a b c)"), R=[tb_va]),
                                   cx.dma("sp", dbg["dbg_kT"], kT[:].rearrange("p a b -> p (a b)"), R=[tb_kT]),
                                   cx.dma("sp", dbg["dbg_kiT"], kiT[:], R=[tb_kiT]),
                                   cx.dma("sp", dbg["dbg_wi"], wi[:].rearrange("p a b -> p (a b)"), R=[tb_wi])]
                        for h in range(H):
                            g = h // HPG
                            e_, tb_e = ee[h % 2]
                            p_, tb_p = pt[h % 2]
                            for kb in range(NKB):
                                k0_, k1_ = kb * 512, min(KW, (kb + 1) * 512)
                                MM(psS[:, k0_:k1_], q_[:, h, :], kT[:, g, k0_:k1_], True, True,
                                   [tb_q, tb_kT], [tb_psQ[kb]])
                            V(lambda: nc.vector.reduce_max(out=sm[:, 1:2], in_=psS[:, 0:KW], axis=AX.X), tb_psQ[:NKB], [tb_sm])
                            vts(sm[:, 2:3], sm[:, 1:2], -scale, None, ALU.mult, None, [tb_sm], [tb_sm])
                            act(e_[:, 0:KW], psS[:, 0:KW], AF.Exp, tb_psQ[:NKB] + [tb_sm], [tb_e], bias=sm[:, 2:3], scale=scale)
                            vtt(e_[:, 0:KW], e_[:, 0:KW], msk[:, 0:KW], ALU.mult, [tb_e, tb_msk], [tb_e])
                            for k0 in range(0, NKT, 8):
                                k1 = min(NKT, k0 + 8)
                                for k in range(k0, k1):
                                    TR(psT[:, (k - k0) * 128:(k - k0 + 1) * 128], e_[:, k * 128:(k + 1) * 128], ident[:],
                                       [tb_e, tb_id], [tb_psT])
                                A(lambda k0=k0, k1=k1: nc.scalar.copy(
                                    out=p_[:, k0:k1, :], in_=psT[:, 0:(k1 - k0) * 128].rearrange("p (k t) -> p k t", t=128)),
                                  [tb_psT], [tb_p])
                            for k in range(NKT):
                                MM(psO[:, 0:129], p_[:, k, :], va[:, k, g, 0:129], k == 0, k == NKT - 1,
                                   [tb_p, tb_va], [tb_psO])
                            V(lambda: nc.vector.reciprocal(out=sm[:, 3:4], in_=psO[:, 128:129]), [tb_psO], [tb_sm])
                            if DBG == "attn0" and tt == 0 and h == 0:
                                V(lambda: nc.vector.tensor_copy(out=wk[:, 0:132], in_=psO[:, 0:132]), [tb_psO], [tb_wk])
                                evs += [cx.dma("sp", dbg["dbg_e"], e_[:], R=[tb_e]), cx.dma("sp", dbg["dbg_sm"], sm[:], R=[tb_sm]),
                                        cx.dma("sp", dbg["dbg_pso"], wk[:, 0:132], R=[tb_wk])]
                            vts(ot[:, h, :], psO[:, 0:128], sm[:, 3:4], None, ALU.mult, None, [tb_psO, tb_sm], [tb_ot])
                        if DBG == "attn0" and tt == 0:
                            evs.append(cx.dma("sp", dbg["dbg_ot"], ot[:].rearrange("p a b -> p (a b)"), R=[tb_ot]))
                            for ev in evs:
                                cx._wait("sp", ev)
                            stopf[0] = True
                            break
                        for h0 in range(0, H, 8):
                            h1 = min(H, h0 + 8)
                            for h in range(h0, h1):
                                TR(psT[:, (h - h0) * 128:(h - h0 + 1) * 128], ot[:, h, :], ident[:], [tb_ot, tb_id], [tb_psT])
                            A(lambda h0=h0, h1=h1: nc.scalar.copy(
                                out=yo[:, h0:h1, :], in_=psT[:, 0:(h1 - h0) * 128].rearrange("p (k t) -> p k t", t=128)),
                              [tb_psT], [tb_yo])
                        cx.dma("sp", yT_d[0:NAC, :, tt * 128:(tt + 1) * 128].rearrange("h p t -> p h t"), yo[:],
                               R=[tb_yo], W=tb_yT[0:NAC])

            if DBG == "attnB":
                stopf[0] = True
            if stopf[0]:
                return
            with scope() as st:
                wc = [sb(st, "wc%d" % i, [128, KC, 128], BF16) for i in range(3)]
                wcn = [0]

                def proj_fm(c0, dst, tb_dst, func=None, own=False):
                    wt, tb_w = wc[wcn[0] % 3]
                    wcn[0] += 1
                    wload(wt, tb_w, W[:, c0:c0 + 128], D, 128)
                    xs, tb_xs = (xq, tb_xq) if own else (xT, tb_xT)
                    for tb_ in range(NBQ if own else NB):
                        ps, tb_ps = psQ[tb_]
                        for k in range(KC):
                            MM(ps, wt[:, k, :], xs[:, k, tb_ * 512:(tb_ + 1) * 512], k == 0, k == KC - 1,
                               [tb_w, tb_xs], [tb_ps])
                        if func is None:
                            A(lambda tb_=tb_, ps=ps: nc.scalar.copy(out=dst[:, tb_ * 512:(tb_ + 1) * 512], in_=ps),
                              [tb_ps], [tb_dst])
                        else:
                            act(dst[:, tb_ * 512:(tb_ + 1) * 512], ps, func, [tb_ps], [tb_dst])

                f = [sb(st, "f%d" % i, [128, T], F32) for i in range(7)]
                hb = [sb(st, "hb%d" % i, [128, T], BF16) for i in range(4)]
                yb, tb_yb = sb(st, "yb", [128, T], BF16)
                col, tb_col = sb(st, "col", [128, 16], F32)
                cw, tb_cw = sb(st, "cw", [128, 4, 128], BF16)
                wa_t, tb_wa = sb(st, "wa_t", [128, 1, 128], BF16)
                wx_t, tb_wx = sb(st, "wx_t", [128, 1, 128], BF16)

                def colvec(j, src_row, c0):
                    cx.dma("sp", col[:, j:j + 1], src_row[c0:c0 + 128].rearrange("(p o) -> p o", o=1), W=[tb_col],
                           allow_slow_non_contiguous=True)

                for g in range(4):
                    win = (2, 4, 8, 16)[g]
                    ncg = PW // 128
                    for i in range(ncg):
                        c = g * ncg + i
                        (p_, tb_p), (a_, tb_a), (b_, tb_b) = f[0], f[1], f[2]
                        proj_fm(O_P + c * 128, p_, tb_p)
                        src, tb_src = p_, tb_p
                        sh = 1
                        flip = 0
                        while sh < win:
                            dst, tb_dst = (a_, tb_a) if flip == 0 else (b_, tb_b)
                            flip ^= 1
                            V(lambda dst=dst, src=src, sh=sh: nc.vector.tensor_copy(out=dst[:, 0:sh], in_=src[:, 0:sh]),
                              [tb_src], [tb_dst])
                            vtt(dst[:, sh:T], src[:, sh:T], src[:, 0:T - sh], ALU.add, [tb_src], [tb_dst])
                            src, tb_src = dst, tb_dst
                            sh *= 2
                        d_, tb_d = (a_, tb_a) if src is b_ else (b_, tb_b)
                        vstt(d_[:, win - 1:T], src[:, win - 1:T], 1.0 / win, p_[:, win - 1:T], ALU.mult, ALU.subtract,
                             [tb_src, tb_p], [tb_d])
                        for t in range(win - 1):
                            vstt(d_[:, t:t + 1], src[:, t:t + 1], 1.0 / (t + 1), p_[:, t:t + 1], ALU.mult, ALU.subtract,
                                 [tb_src, tb_p], [tb_d])
                        if split:
                            blend(hb[i][0][:, 0:TQ], d_[:], TQ, [tb_d], [hb[i][1]])
                        else:
                            V(lambda d_=d_, i=i: nc.vector.tensor_copy(out=hb[i][0][:], in_=d_[:]), [tb_d], [hb[i][1]])
                    for j in range(ncg):
                        c = g * ncg + j
                        wload(cw, tb_cw, pool_w[l, g][:, j * 128:(j + 1) * 128], PW, 128)
                        colvec(0, pool_scale[l], c * 128)
                        gt_, tb_gt = f[3]
                        proj_fm(O_GT + (NAC + c) * 128, gt_, tb_gt, AF.Sigmoid, own=True)
                        for tb_ in range(NBQ):
                            ps, tb_ps = psAB[tb_ % 2]
                            for i in range(ncg):
                                MM(ps[:, :], cw[:, i, :], hb[i][0][:, tb_ * 512:(tb_ + 1) * 512], i == 0, i == ncg - 1,
                                   [tb_cw, hb[i][1]], [tb_ps])
                            vstt(yb[:, tb_ * 512:(tb_ + 1) * 512], ps[:, :], col[:, 0:1], gt_[:, tb_ * 512:(tb_ + 1) * 512],
                                 ALU.mult, ALU.mult, [tb_ps, tb_col, tb_gt], [tb_yb])
                        cx.dma("sp", yT_d[NAC + c][:, 0:TQ], yb[:, 0:TQ], R=[tb_yb], W=[tb_yT[NAC + c]])
                for n in range(KC):
                    (xr, tb_xr), (xc, tb_xc), (r_, tb_r), (i_, tb_i), (a_, tb_a), (g_, tb_g), (gt_, tb_gt) = f
                    xcb, tb_xcb = hb[0]
                    proj_fm(O_XR + n * 128, xr, tb_xr)
                    for tap in range(4):
                        colvec(tap, conv_w[l, tap], n * 128)
                    colvec(4, conv_b[l], n * 128)
                    colvec(5, lru_ba[l], n * 128)
                    colvec(6, lru_bx[l], n * 128)
                    colvec(7, lru_lam[l], n * 128)
                    act(col[:, 8:9], col[:, 7:8], AF.Exp, [tb_col], [tb_col], scale=-1.0)
                    act(col[:, 9:10], col[:, 8:9], AF.Ln, [tb_col], [tb_col], bias=1.0, scale=1.0)
                    vts(col[:, 10:11], col[:, 9:10], -8.0, None, ALU.mult, None, [tb_col], [tb_col])
                    vts(xc[:], xr[:], col[:, 3:4], col[:, 4:5], ALU.mult, ALU.add, [tb_xr, tb_col], [tb_xc])
                    for tap in range(3):
                        sh = 3 - tap
                        vstt(xc[:, sh:T], xr[:, 0:T - sh], col[:, tap:tap + 1], xc[:, sh:T], ALU.mult, ALU.add,
                             [tb_xr, tb_col, tb_xc], [tb_xc])
                    V(lambda: nc.vector.tensor_copy(out=xcb[:], in_=xc[:]), [tb_xc], [tb_xcb])
                    wload(wa_t, tb_wa, lru_wa[l, n], 128, 128)
                    wload(wx_t, tb_wx, lru_wx[l, n], 128, 128)
                    for tb_ in range(NB):
                        tsl = slice(tb_ * 512, (tb_ + 1) * 512)
                        psa, tb_psa = psAB[0]
                        psb, tb_psb = psAB[1]
                        MM(psa[:, :], wa_t[:, 0, :], xcb[:, tsl], True, True, [tb_wa, tb_xcb], [tb_psa])
                        MM(psb[:, :], wx_t[:, 0, :], xcb[:, tsl], True, True, [tb_wx, tb_xcb], [tb_psb])
                        act(r_[:, tsl], psa[:, :], AF.Sigmoid, [tb_psa, tb_col], [tb_r], bias=col[:, 5:6], scale=1.0)
                        act(i_[:, tsl], psb[:, :], AF.Sigmoid, [tb_psb, tb_col], [tb_i], bias=col[:, 6:7], scale=1.0)
                    act(a_[:], r_[:], AF.Exp, [tb_r, tb_col], [tb_a], scale=col[:, 10:11])
                    vtt(r_[:], a_[:], a_[:], ALU.mult, [tb_a], [tb_r])
                    vts(r_[:], r_[:], -1.0, 1.0, ALU.mult, ALU.add, [tb_r], [tb_r])
                    vts(r_[:], r_[:], 0.0, None, ALU.max, None, [tb_r], [tb_r])
                    act(r_[:], r_[:], AF.Sqrt, [tb_r], [tb_r])
                    vtt(i_[:], i_[:], xc[:], ALU.mult, [tb_i, tb_xc], [tb_i])
                    vtt(i_[:], i_[:], r_[:], ALU.mult, [tb_i, tb_r], [tb_i])
                    V(lambda: nc.vector.tensor_tensor_scan(out=xc[:], data0=a_[:], data1=i_[:], initial=0.0,
                                                           op0=ALU.mult, op1=ALU.add), [tb_a, tb_i], [tb_xc])
                    if split:
                        blend(a_[:, 0:TQ], xc[:], TQ, [tb_xc], [tb_a])
                        hq, tb_hq = a_, tb_a
                    else:
                        hq, tb_hq = xc, tb_xc
                    q_ = slice(0, TQ)
                    proj_fm(O_GR + n * 128, g_, tb_g, own=True)
                    vtt(r_[:, q_], g_[:, q_], g_[:, q_], ALU.mult, [tb_g], [tb_r])
                    vts(r_[:, q_], r_[:, q_], 0.044715, 1.0, ALU.mult, ALU.add, [tb_r], [tb_r])
                    vtt(r_[:, q_], r_[:, q_], g_[:, q_], ALU.mult, [tb_r, tb_g], [tb_r])
                    act(r_[:, q_], r_[:, q_], AF.Sigmoid, [tb_r], [tb_r], scale=float(2.0 * np.sqrt(2.0 / np.pi)))
                    vtt(r_[:, q_], r_[:, q_], g_[:, q_], ALU.mult, [tb_r, tb_g], [tb_r])
                    vtt(r_[:, q_], r_[:, q_], hq[:, q_], ALU.mult, [tb_r, tb_hq], [tb_r])
                    proj_fm(O_GT + (NAC + KC + n) * 128, gt_, tb_gt, AF.Sigmoid, own=True)
                    vtt(yb[:, q_], r_[:, q_], gt_[:, q_], ALU.mult, [tb_r, tb_gt], [tb_yb])
                    cx.dma("sp", yT_d[NAC + KC + n][:, q_], yb[:, q_], R=[tb_yb], W=[tb_yT[NAC + KC + n]])
                for c in range(NAC):
                    gt_, tb_gt = f[3]
                    ya, tb_ya = hb[c % 2]
                    proj_fm(O_GT + c * 128, gt_, tb_gt, AF.Sigmoid, own=True)
                    cx.dma("sp", ya[:, 0:TQ], yT_d[c][:, 0:TQ], R=[tb_yT[c]], W=[tb_ya])
                    vtt(yb[:, 0:TQ], ya[:, 0:TQ], gt_[:, 0:TQ], ALU.mult, [tb_ya, tb_gt], [tb_yb])
                    cx.dma("sp", yT_d[c][:, 0:TQ], yb[:, 0:TQ], R=[tb_yb], W=[tb_yT[c]])
                for c in range(NMC):
                    ya, tb_ya = hb[c % 4]
                    cx.dma("sp", ya[:, 0:TQ], yT_d[c][:, 0:TQ], R=[tb_yT[c]], W=[tb_ya])
                    cx.dma("sp", mT_d[0:NTQ, :, c, :].rearrange("t p q -> p t q"),
                           ya[:, 0:TQ].rearrange("p (t q) -> p t q", q=128), R=[tb_ya], W=[tb_mT])

            with scope() as st:
                wo, tb_wo = sb(st, "wo", [128, NMC, 512], BF16)
                mt = [sb(st, "mt%d" % i, [128, NMC, 128], BF16) for i in range(2)]
                po = [sb(st, "po%d" % i, [128, 512], F32) for i in range(2)]
                for nb in range(D // 512):
                    wload(wo, tb_wo, w_out[l][:, nb * 512:(nb + 1) * 512], MIXW, 512)
                    for tt in range(NTQ):
                        m_, tb_m = mt[tt % 2]
                        o_, tb_o = po[tt % 2]
                        ps, tb_ps = psAB[tt % 2]
                        cx.dma("sp", m_[:], mT_d[tt], R=[tb_mT], W=[tb_m])
                        for c in range(NMC):
                            MM(ps[:, :], m_[:, c, :], wo[:, c, :], c == 0, c == NMC - 1, [tb_m, tb_wo], [tb_ps])
                        A(lambda o_=o_, ps=ps: nc.scalar.copy(out=o_[:], in_=ps[:, :]), [tb_ps], [tb_o])
                        cx.dma("sp", pre_d[tt * 128:(tt + 1) * 128, nb * 512:(nb + 1) * 512], o_[:], R=[tb_o],
                               W=[tb_pre[tt]])

        def ffn(wg, wu, wd, F, first, scale_e, xq, tb_xq, TQ, GF, yacc=None, tb_yacc=None):
            nfc = (F + 127) // 128
            assert F % 128 == 0 and GF % 4 == 0
            NTQ, NBQ = TQ // 128, TQ // 512
            with scope() as st:
                wgc = [sb(st, "wgc%d" % i, [128, KC, 512], BF16) for i in range(2)]
                wuc = [sb(st, "wuc%d" % i, [128, KC, 512], BF16) for i in range(2)]
                hT, tb_hT = sb(st, "hT", [128, GF, TQ], BF16)
                wdb = [sb(st, "wdb%d" % i, [128, GF, 512], BF16) for i in range(2)]
                sg = [sb(st, "sg%d" % i, [128, 512], F32) for i in range(2)]
                po = [sb(st, "fpo%d" % i, [128, 512], F32) for i in range(2)]
                c3 = [0]
                items = []
                hcnt = 0
                for f0 in range(0, nfc, GF):
                    f1 = min(nfc, f0 + GF)
                    for fb in range(f0, f1, 4):
                        items.append(("H", f0, f1, fb, hcnt))
                        hcnt += 1
                    for nb in range(D // 512):
                        items.append(("D", f0, f1, nb, 0))

                def load(it):
                    kind, f0, f1, j, hc = it
                    if kind == "H":
                        n = (min(f1, j + 4) - j) * 128
                        g_, tb_g = wgc[hc % 2]
                        u_, tb_u = wuc[hc % 2]
                        wload(g_, tb_g, wg[:, j * 128:j * 128 + n], D, n)
                        wload(u_, tb_u, wu[:, j * 128:j * 128 + n], D, n)
                    else:
                        w_, tb_w = wdb[j % 2]
                        wload(w_, tb_w, wd[f0 * 128:f1 * 128, j * 512:(j + 1) * 512], (f1 - f0) * 128, 512)

                def compute(it):
                    kind, f0, f1, j, hc = it
                    if kind == "H":
                        g_, tb_g = wgc[hc % 2]
                        u_, tb_u = wuc[hc % 2]
                        for fi in range(j, min(f1, j + 4)):
                            cs = slice((fi - j) * 128, (fi - j + 1) * 128)
                            for tb_ in range(NBQ):
                                tsl = slice(tb_ * 512, (tb_ + 1) * 512)
                                psa, tb_psa = psQ[(2 * tb_) % 4]
                                psb, tb_psb = psQ[(2 * tb_ + 1) % 4]
                                s_, tb_s = sg[tb_ % 2]
                                for k in range(KC):
                                    MM(psa, g_[:, k, cs], xq[:, k, tsl], k == 0, k == KC - 1, [tb_g, tb_xq], [tb_psa])
                                for k in range(KC):
                                    MM(psb, u_[:, k, cs], xq[:, k, tsl], k == 0, k == KC - 1, [tb_u, tb_xq], [tb_psb])
                                act(s_[:, :], psa, AF.Silu, [tb_psa], [tb_s])
                                vtt(hT[:, fi - f0, tsl], s_[:, :], psb, ALU.mult, [tb_s, tb_psb], [tb_hT])
                    else:
                        nb = j
                        w_, tb_w = wdb[nb % 2]
                        for tt in range(NTQ):
                            ps, tb_ps = psAB[c3[0] % 2]
                            o_, tb_o = po[c3[0] % 2]
                            c3[0] += 1
                            for fi in range(f0, f1):
                                MM(ps[:, :], hT[:, fi - f0, tt * 128:(tt + 1) * 128], w_[:, fi - f0, :], fi == f0,
                                   fi == f1 - 1, [tb_hT, tb_w], [tb_ps])
                            if yacc is not None:
                                dv = yacc[:, tt, nb * 512:(nb + 1) * 512]
                                if first and f0 == 0:
                                    if scale_e is None:
                                        A(lambda dv=dv, ps=ps: nc.scalar.copy(out=dv, in_=ps[:, :]), [tb_ps], [tb_yacc[tt]])
                                    else:
                                        vts(dv, ps[:, :], rw[:, tt, scale_e:scale_e + 1], None, ALU.mult, None,
                                            [tb_ps, tb_rw], [tb_yacc[tt]])
                                elif scale_e is None:
                                    vtt(dv, dv, ps[:, :], ALU.add, [tb_ps, tb_yacc[tt]], [tb_yacc[tt]])
                                else:
                                    vstt(dv, ps[:, :], rw[:, tt, scale_e:scale_e + 1], dv, ALU.mult, ALU.add,
                                         [tb_ps, tb_rw, tb_yacc[tt]], [tb_yacc[tt]])
                                continue
                            if scale_e is None:
                                A(lambda o_=o_, ps=ps: nc.scalar.copy(out=o_[:], in_=ps[:, :]), [tb_ps], [tb_o])
                            else:
                                vts(o_[:], ps[:, :], rw[:, tt, scale_e:scale_e + 1], None, ALU.mult, None,
                                    [tb_ps, tb_rw], [tb_o])
                            dst = yf_d[tt * 128:(tt + 1) * 128, nb * 512:(nb + 1) * 512]
                            if first and f0 == 0:
                                cx.dma("pool", dst, o_[:], R=[tb_o], W=[tb_yf[tt]])
                            else:
                                cx.dma("pool", dst, o_[:], R=[tb_o], W=[tb_yf[tt]], accum_op=ALU.add)

                load(items[0])
                for ii, it in enumerate(items):
                    if ii + 1 < len(items):
                        load(items[ii + 1])
                    compute(it)


        def drain_all():
            for tbl in (tb_xres, tb_pre, tb_yf, tb_yT, tb_qT, tb_qiT, [tb_mT]):
                for b in tbl:
                    for ev in list(b.w.values()):
                        cx._wait("sp", ev)

        for s in range(NSEQ):
            with scope() as st:
                load_input_stage(st, s)
            for l in range(L):
                split = SPLIT > 1 and l == L - 1
                ntq_l = (TQL // 128) if split else NT
                mixer(l, s, split)
                if stopf[0]:
                    drain_all()
                    return nc
                if DBG == "mixer%d" % l:
                    drain_all()
                    return nc
                with contextlib.ExitStack() as tl:
                    if split:
                        cx.barrier()
                        xst.close()
                        xs_, tb_xs_ = sb(tl, "xq2", [128, KC, TQL], BF16)
                        yacc, _ = sb(tl, "yacc", [128, TQL // 128, D], F32)
                        tb_yacc = [TB() for _ in range(TQL // 128)]
                        tq_l, gf_l = TQL, 8
                    else:
                        xs_, tb_xs_ = xT, tb_xT
                        yacc, tb_yacc = None, None
                        tq_l, gf_l = T, 8
                    with scope() as st:
                        layer_norm_stage(st, "pre", ln_mix_g[l], ln_mix_b[l],
                                         router=(routerT[l // 2] if l % 2 == 1 else None), ntq=ntq_l, res_blend=split,
                                         xdst=xs_, tb_xdst=tb_xs_)
                    if l % 2 == 0:
                        ffn(dw_gate[l // 2], dw_up[l // 2], dw_down[l // 2], DFF, True, None, xs_, tb_xs_, tq_l, gf_l,
                            yacc, tb_yacc)
                    else:
                        for e in range(NE):
                            ffn(mw_gate[l // 2, e], mw_up[l // 2, e], mw_down[l // 2, e], DEXP, e == 0, e,
                                xs_, tb_xs_, tq_l, gf_l, yacc, tb_yacc)
                    with scope() as st:
                        layer_norm_stage(st, "yf", ln_ffn_g[l], ln_ffn_b[l],
                                         dst_final=(y_out[s] if l == L - 1 else None), ntq=ntq_l,
                                         add_sb=yacc, tb_add_sb=tb_yacc)
                    cx.barrier()
        for ev in cx.last_out:
            cx._wait("sp", ev)
        xst.close()
    return nc


def consts():
    ident = np.eye(128, dtype=np.float32)
    inva = np.tile((10000.0 ** (-np.arange(0, 128, 2, dtype=np.float32) / 128.0)).astype(np.float32)[None, :], (128, 1))
    invi = np.tile((10000.0 ** (-np.arange(0, 64, 2, dtype=np.float32) / 64.0)).astype(np.float32)[None, :], (128, 1))
    return ident, inva, invi


def run(cfg, inputs):
    ncores = cfg["NCORES"]
    split = cfg.get("SPLIT", 1)
    nseq = cfg["B"] * split // ncores
    T = cfg["T"]
    tq = T // split
    nt = T // 128
    nc = build_nc(cfg)
    ident, inva, invi = consts()
    rows = np.arange(128)[:, None] + 128 * np.arange(nt)[None, :]
    limf = ((rows // 64 + 1) * 64).astype(np.float32)
    in_maps = []
    for c in range(ncores):
        b0, hf = (c // split) * nseq, c % split
        m = {}
        for k, v in inputs.items():
            v = np.asarray(v)
            if k in ("x", "positions"):
                m[k] = np.ascontiguousarray(v[b0:b0 + nseq])
            elif k == "moe_router":
                m["moe_routerT"] = np.ascontiguousarray(np.swapaxes(v, 1, 2))
            else:
                m[k] = v
        pos = np.asarray(inputs["positions"])[b0:b0 + nseq]
        if split == 1:
            m["positions_q"] = np.ascontiguousarray(pos)
        else:
            m["positions_q"] = np.ascontiguousarray(pos.reshape(nseq, nt // split, split, 128)[:, :, hf, :].reshape(nseq, tq))
        sel = np.zeros((128, 2), np.float32)
        sel[:, hf] = 1.0
        m["c_sel"] = sel
        m["c_limf"] = limf
        gl = (np.arange(nt)[None, :] * split + hf) * 128 + np.arange(128)[:, None]
        m["c_limq"] = ((gl // 64 + 1) * 64).astype(np.float32)
        m["c_ident"], m["c_inva"], m["c_invi"] = ident, inva, invi
        in_maps.append(m)
    if cfg.get("ONLY_MAPS"):
        return nc, in_maps
    if cfg.get("SUBSET") is not None:
        sub = cfg["SUBSET"]
        r = run_bass_kernel_spmd(nc, [in_maps[c] for c in sub], core_ids=list(range(len(sub))))
        return r.results
    res = run_bass_kernel_spmd(nc, in_maps, core_ids=list(range(ncores)))
    if cfg.get("STOP"):
        return res.results
    out = np.empty((cfg["B"], T, cfg["D"]), np.float32)
    for c in range(ncores):
        b0, hf = (c // split) * nseq, c % split
        if split == 1:
            out[b0:b0 + nseq] = res.results[c]["y"]
        else:
            out[b0].reshape(nt // split, split, 128, cfg["D"])[:, hf] = res.results[c]["y"][0].reshape(nt // split, 128, cfg["D"])
    return out


def kernel(**inputs):
    return run(FULL, inputs).astype(np.float32)
```

```python
import contextlib
import numpy as np
import ml_dtypes
import concourse.bass as bass
import concourse.mybir as mybir
from concourse.bass_utils import run_bass_kernel_spmd

F32 = mybir.dt.float32
BF16 = mybir.dt.bfloat16
I32 = mybir.dt.int32
AF = mybir.ActivationFunctionType
ALU = mybir.AluOpType
AX = mybir.AxisListType

FULL = dict(D=2048, T=2048, B=4, L=2, H=16, G=4, HI=16, TOPK=256, DFF=5504, NE=8, DEXP=7168, NCORES=8, SPLIT=2)
LN_EPS = 1e-5
LIM_SEM = 24000
NEG1 = -1.0e30
NEG2 = -3.0e38
MAGIC = 12582912.0
TWO_PI = 6.283185307179586
C1 = 6.28125
C2 = TWO_PI - 6.28125


class TB:
    __slots__ = ("w", "weng", "r")

    def __init__(self):
        self.w = {}
        self.weng = None
        self.r = {}


class Ctx:
    def __init__(self, nc, st):
        self.nc = nc
        self.st = st
        self.nsem = 0
        self.LIM = LIM_SEM
        self.eng = {"pe": nc.tensor, "act": nc.scalar, "dve": nc.vector, "pool": nc.gpsimd, "sp": nc.sync}
        self.sem = {}
        self.cnt = {}
        self.seen = {k: {} for k in self.eng}
        for k in ("pe", "act", "dve", "pool"):
            self.sem[k] = st.enter_context(nc.semaphore("s_" + k))
            self.cnt[k] = 0
        self.NS = 8
        self.dsem = {}
        self.di = {}
        for q in ("sp", "pool", "act"):
            self.dsem[q] = [st.enter_context(nc.semaphore("d_%s%d" % (q, i))) for i in range(self.NS)]
            self.di[q] = 0
        self.semid = {}
        self.last_out = []

    def _wait(self, e, ev):
        if ev is None:
            return
        sem, val = ev
        k = id(sem)
        if self.seen[e].get(k, 0) >= val:
            return
        self.eng[e].wait_ge(sem, val)
        self.seen[e][k] = val

    def _deps(self, e, R, W, acc_ok=False):
        for b in R:
            for ev in b.w.values():
                self._wait(e, ev)
        for b in W:
            for k, ev in b.w.items():
                if not (acc_ok and k == "pe"):
                    self._wait(e, ev)
            for ev in b.r.values():
                self._wait(e, ev)

    def _mark(self, key, e, ev, R, W):
        for b in R:
            b.r[key] = ev
        for b in W:
            b.w[key] = ev
            b.r = {}

    def op(self, e, fn, R=(), W=(), acc_ok=False):
        self._deps(e, R, W, acc_ok)
        ins = fn()
        self.cnt[e] += 1
        ins.then_inc(self.sem[e], 1)
        ev = (self.sem[e], self.cnt[e])
        self._mark(e, e, ev, R, W)
        return ev

    def barrier(self):
        evs = [(self.sem[k], self.cnt[k]) for k in ("pe", "act", "dve", "pool") if self.cnt[k] > 0]
        for q in self.dsem:
            di = self.di[q]
            for j in range(self.NS):
                if di > j:
                    evs.append((self.dsem[q][j], 16 * ((di - j - 1) // self.NS + 1)))
        for e in ("pe", "act", "dve", "pool", "sp"):
            for ev in evs:
                self._wait(e, ev)
        for k in ("pe", "act", "dve", "pool"):
            if self.cnt[k] > self.LIM:
                self.nsem += 1
                self.sem[k] = self.st.enter_context(self.nc.semaphore("s_%s_%d" % (k, self.nsem)))
                self.cnt[k] = 0
        for q in self.dsem:
            if 16 * (self.di[q] // self.NS + 1) > self.LIM:
                self.nsem += 1
                self.dsem[q] = [self.st.enter_context(self.nc.semaphore("d_%s%d_%d" % (q, i, self.nsem)))
                                for i in range(self.NS)]
                self.di[q] = 0

    def dma(self, q, out, in_, R=(), W=(), **kw):
        self._deps(q, R, W)
        i = self.di[q]
        sem = self.dsem[q][i % self.NS]
        if i >= self.NS:
            self._wait(q, (sem, 16 * (i // self.NS)))
        ins = self.eng[q].dma_start(out=out, in_=in_, **kw)
        ins.then_inc(sem, 16)
        ev = (sem, 16 * (i // self.NS + 1))
        self.di[q] = i + 1
        self._mark("dma_%s_%d" % (q, i % self.NS), "dma", ev, R, W)
        return ev


def build_nc(cfg):
    D, T, L, H, G, HI = cfg["D"], cfg["T"], cfg["L"], cfg["H"], cfg["G"], cfg["HI"]
    TOPK, DFF, NE, DEXP = cfg["TOPK"], cfg["DFF"], cfg["NE"], cfg["DEXP"]
    SPLIT = cfg.get("SPLIT", 1)
    NSEQ = cfg["B"] * SPLIT // cfg["NCORES"]
    assert SPLIT == 1 or NSEQ == 1
    KC, NT, NB = D // 128, T // 128, T // 512
    TQL = T // SPLIT
    HPG = H // G
    AW, KW, IW = H * 128, G * 128, HI * 64
    MIXW = AW + 2 * D
    O_Q, O_K, O_V = 0, AW, AW + KW
    O_QI = AW + 2 * KW
    O_KI = O_QI + IW
    O_WI = O_KI + 64
    O_P = O_WI + HI
    O_XR = O_P + D
    O_GR = O_XR + D
    O_GT = O_GR + D
    INC = O_GT + MIXW
    NMC = MIXW // 128
    NAC = AW // 128
    ALPHA = (2 * L) ** 0.25
    NR = TOPK // 8
    PW = D // 4

    nc = bass.Bass("TRN2", target_bir_lowering=False)

    def din(name, shape, dt=F32):
        return nc.dram_tensor(name, list(shape), dt, kind="ExternalInput").ap()

    x_in = din("x", [NSEQ, T, D])
    pos_in = din("positions", [NSEQ, T], I32)
    posq_in = din("positions_q", [NSEQ, TQL], I32)
    sel_in = din("c_sel", [128, 2])
    limf_in = din("c_limf", [128, NT])
    limq_in = din("c_limq", [128, NT])
    w_in = din("mix_w_in", [L, D, INC])
    w_out = din("mix_w_out", [L, MIXW, D])
    pool_w = din("pool_w", [L, 4, PW, PW])
    pool_scale = din("pool_scale", [L, D])
    conv_w = din("conv_w", [L, 4, D])
    conv_b = din("conv_b", [L, D])
    lru_wa = din("lru_wa", [L, KC, 128, 128])
    lru_ba = din("lru_ba", [L, D])
    lru_wx = din("lru_wx", [L, KC, 128, 128])
    lru_bx = din("lru_bx", [L, D])
    lru_lam = din("lru_lam", [L, D])
    ln_mix_g = din("ln_mix_g", [L, D])
    ln_mix_b = din("ln_mix_b", [L, D])
    ln_ffn_g = din("ln_ffn_g", [L, D])
    ln_ffn_b = din("ln_ffn_b", [L, D])
    dw_gate = din("dense_w_gate", [(L + 1) // 2, D, DFF])
    dw_up = din("dense_w_up", [(L + 1) // 2, D, DFF])
    dw_down = din("dense_w_down", [(L + 1) // 2, DFF, D])
    routerT = din("moe_routerT", [max(L // 2, 1), NE, D])
    mw_gate = din("moe_w_gate", [max(L // 2, 1), NE, D, DEXP])
    mw_up = din("moe_w_up", [max(L // 2, 1), NE, D, DEXP])
    mw_down = din("moe_w_down", [max(L // 2, 1), NE, DEXP, D])
    ident_in = din("c_ident", [128, 128])
    inva_in = din("c_inva", [128, 64])
    invi_in = din("c_invi", [128, 32])
    y_out = nc.dram_tensor("y", [NSEQ, TQL, D], F32, kind="ExternalOutput").ap()

    DBG = cfg.get("STOP")

    def dscr(name, shape, dt):
        if DBG and name in ("xres_d", "pre_d", "yf_d", "yT_d", "qT_d", "qiT_d"):
            return nc.dram_tensor(name, list(shape), dt, kind="ExternalOutput").ap()
        return nc.dram_tensor(name, list(shape), dt, kind="Internal").ap()

    xres_d = dscr("xres_d", [T, D], F32)
    pre_d = dscr("pre_d", [T, D], F32)
    yf_d = dscr("yf_d", [T, D], F32)
    yT_d = dscr("yT_d", [NMC, 128, T], BF16)
    mT_d = dscr("mT_d", [NT, 128, NMC, 128], BF16)
    qT_d = dscr("qT_d", [NT, 128, H, 128], BF16)
    qiT_d = dscr("qiT_d", [NT, 128, HI // 2, 128], BF16)
    tb_xres = [TB() for _ in range(NT)]
    tb_pre = [TB() for _ in range(NT)]
    tb_yf = [TB() for _ in range(NT)]
    tb_yT = [TB() for _ in range(NMC)]
    tb_mT = TB()
    tb_qT = [TB() for _ in range(NT)]
    tb_qiT = [TB() for _ in range(NT)]

    class _Stop(Exception):
        pass

    with contextlib.ExitStack() as gst:
        cx = Ctx(nc, gst)
        stopf = [False]

        @contextlib.contextmanager
        def scope():
            with contextlib.ExitStack() as st_:
                yield st_
                cx.barrier()

        uid = [0]

        def sb(st, name, shape, dt):
            uid[0] += 1
            return st.enter_context(nc.sbuf_tensor("%s_%d" % (name, uid[0]), list(shape), dt)), TB()

        def pp(st, name, shape, dt):
            return st.enter_context(nc.psum_tensor(name, list(shape), dt)), TB()

        V = lambda fn, R=(), W=(): cx.op("dve", fn, R, W)
        A = lambda fn, R=(), W=(): cx.op("act", fn, R, W)
        P = lambda fn, R=(), W=(): cx.op("pool", fn, R, W)

        def MM(out, lhsT, rhs, start, stop, R, W):
            return cx.op("pe", lambda: nc.tensor.matmul(out, lhsT, rhs, start=start, stop=stop), R, W, acc_ok=not start)

        def TR(out, in_, ident, R, W):
            return cx.op("pe", lambda: nc.tensor.transpose(out, in_, ident), R, W, acc_ok=True)

        def act(out, in_, func, R, W, **kw):
            return A(lambda: nc.scalar.activation(out=out, in_=in_, func=func, **kw), R, W)

        def vts(out, in0, s1, s2, op0, op1, R, W):
            if op1 is None:
                return V(lambda: nc.vector.tensor_scalar(out=out, in0=in0, scalar1=s1, scalar2=None, op0=op0), R, W)
            return V(lambda: nc.vector.tensor_scalar(out=out, in0=in0, scalar1=s1, scalar2=s2, op0=op0, op1=op1), R, W)

        def vtt(out, in0, in1, op, R, W):
            return V(lambda: nc.vector.tensor_tensor(out=out, in0=in0, in1=in1, op=op), R, W)

        def vstt(out, in0, sc, in1, op0, op1, R, W):
            return V(lambda: nc.vector.scalar_tensor_tensor(out=out, in0=in0, scalar=sc, in1=in1, op0=op0, op1=op1), R, W)

        def wload(dst, dtb, src2d, rows, cols):
            nk = rows // 128
            for k0 in range(0, nk, 8):
                k1 = min(nk, k0 + 8)
                cx.dma("pool", dst[:, k0:k1, 0:cols],
                       src2d[k0 * 128:k1 * 128, :].rearrange("(k p) n -> p k n", p=128), W=[dtb])

        def bcast_row(dst, dtb, src_row, n):
            cx.dma("sp", dst, src_row.partition_broadcast(128), W=[dtb])

        ident_f, tb_idf = sb(gst, "ident_f", [128, 128], F32)
        ident, tb_id = sb(gst, "ident", [128, 128], BF16)
        inva, tb_inva = sb(gst, "inva", [128, 64], F32)
        invi, tb_invi = sb(gst, "invi", [128, 32], F32)
        rw, tb_rw = sb(gst, "rw", [128, NT, NE], F32)
        selt, tb_sel = sb(gst, "selt", [128, 2], F32)
        limf, tb_limf = sb(gst, "limf", [128, NT], F32)
        limq, tb_limq = sb(gst, "limq", [128, NT], F32)
        cx.dma("sp", selt[:], sel_in, W=[tb_sel])
        cx.dma("sp", limf[:], limf_in, W=[tb_limf])
        cx.dma("sp", limq[:], limq_in, W=[tb_limq])
        xst = contextlib.ExitStack()
        xT, tb_xT = sb(xst, "xT", [128, KC, T], BF16)

        def blend(dst, src, tq, R, W):
            sv = src.rearrange("p (j two q) -> p j two q", two=2, q=128)
            dv = dst.rearrange("p (j q) -> p j q", q=128)
            vts(dv, sv[:, :, 0, :], selt[:, 0:1], None, ALU.mult, None, R + [tb_sel], W)
            vstt(dv, sv[:, :, 1, :], selt[:, 1:2], dv, ALU.mult, ALU.add, R + [tb_sel] + W, W)
        cx.dma("sp", ident_f[:], ident_in, W=[tb_idf])
        cx.dma("sp", inva[:], inva_in, W=[tb_inva])
        cx.dma("sp", invi[:], invi_in, W=[tb_invi])
        V(lambda: nc.vector.tensor_copy(out=ident[:], in_=ident_f[:]), [tb_idf], [tb_id])

        psS, tb_psS = pp(gst, "psS", [128, 2048], F32)
        psA, tb_psA = pp(gst, "psA", [128, 512], F32)
        psB, tb_psB = pp(gst, "psB", [128, 512], F32)
        psT, tb_psT = pp(gst, "psT", [128, 1024], BF16)
        psO, tb_psO = pp(gst, "psO", [128, 512], F32)
        psAB = [(psA, tb_psA), (psB, tb_psB)]
        psQ = [(psS[:, i * 512:(i + 1) * 512], TB()) for i in range(4)]
        tb_psQ = [q[1] for q in psQ]

        def transpose_tile_to_xT(src_bf, tb_src, tt, st_tmp, xT=xT, tb_xT=tb_xT):
            for k0 in range(0, KC, 8):
                k1 = min(KC, k0 + 8)
                for k in range(k0, k1):
                    TR(psT[:, (k - k0) * 128:(k - k0 + 1) * 128], src_bf[:, k * 128:(k + 1) * 128], ident[:],
                       [tb_src, tb_id], [tb_psT])
                A(lambda: nc.scalar.copy(out=xT[:, k0:k1, tt * 128:(tt + 1) * 128],
                                         in_=psT[:, 0:(k1 - k0) * 128].rearrange("p (k t) -> p k t", t=128)),
                  [tb_psT], [tb_xT])

        def layer_norm_stage(st, src_kind, g_row, b_row, dst_final=None, router=None, ntq=None, res_blend=False,
                             xdst=None, tb_xdst=None, add_sb=None, tb_add_sb=None):
            ntq = NT if ntq is None else ntq
            if xdst is None:
                xdst, tb_xdst = xT, tb_xT
            gt, tb_g = sb(st, "ln_g", [128, D], F32)
            bt, tb_b = sb(st, "ln_b", [128, D], F32)
            bcast_row(gt[:], tb_g, g_row, D)
            bcast_row(bt[:], tb_b, b_row, D)
            if router is not None:
                wr, tb_wr = sb(st, "wr", [128, NE, D], F32)
                for e in range(NE):
                    cx.dma("sp", wr[:, e, :], router[e].partition_broadcast(128), W=[tb_wr])
            nst = (D + 511) // 512
            bufs = []
            for i in range(1 if router is not None else 2):
                xa, tb_xa = sb(st, "ln_xa%d" % i, [128, D], F32)
                xb, tb_xb = sb(st, "ln_xb%d" % i, [128, D], F32)
                xh, tb_xh = sb(st, "ln_xh%d" % i, [128, D], BF16)
                stt, tb_st = sb(st, "ln_st%d" % i, [128, nst, 6], F32)
                mv, tb_mv = sb(st, "ln_mv%d" % i, [128, 8], F32)
                lg, tb_lg = sb(st, "ln_lg%d" % i, [128, 16], F32)
                bufs.append((xa, tb_xa, xb, tb_xb, xh, tb_xh, stt, tb_st, mv, tb_mv, lg, tb_lg))
            add_d, tb_add = (pre_d, tb_pre) if src_kind == "pre" else (yf_d, tb_yf)
            for tt in range(ntq):
                xa, tb_xa, xb, tb_xb, xh, tb_xh, stt, tb_st, mv, tb_mv, lg, tb_lg = bufs[tt % len(bufs)]
                rs = slice(tt * 128, (tt + 1) * 128)
                if res_blend:
                    rs1 = slice((2 * tt) * 128, (2 * tt + 1) * 128)
                    rs2 = slice((2 * tt + 1) * 128, (2 * tt + 2) * 128)
                    cx.dma("sp", xa[:], xres_d[rs1, :], R=[tb_xres[2 * tt]], W=[tb_xa])
                    cx.dma("sp", xb[:], xres_d[rs2, :], R=[tb_xres[2 * tt + 1]], W=[tb_xb])
                else:
                    cx.dma("sp", xa[:], xres_d[rs, :], R=[tb_xres[tt]], W=[tb_xa])
                if res_blend:
                    vts(xa[:], xa[:], selt[:, 0:1], None, ALU.mult, None, [tb_xa, tb_sel], [tb_xa])
                    vstt(xa[:], xb[:], selt[:, 1:2], xa[:], ALU.mult, ALU.add, [tb_xa, tb_xb, tb_sel], [tb_xa])
                if add_sb is not None:
                    vstt(xa[:], xa[:], ALPHA, add_sb[:, tt, :], ALU.mult, ALU.add, [tb_xa, tb_add_sb[tt]], [tb_xa])
                else:
                    cx.dma("sp", xb[:], add_d[rs, :], R=[tb_add[tt]], W=[tb_xb])
                    vstt(xa[:], xa[:], ALPHA, xb[:], ALU.mult, ALU.add, [tb_xa, tb_xb], [tb_xa])
                for j in range(nst):
                    V(lambda j=j: nc.vector.bn_stats(out=stt[:, j, :], in_=xa[:, j * 512:min(D, (j + 1) * 512)]),
                      [tb_xa], [tb_st])
                V(lambda: nc.vector.bn_aggr(out=mv[:, 0:2], in_=stt[:].rearrange("p a b -> p (a b)")), [tb_st], [tb_mv])
                vts(mv[:, 2:3], mv[:, 1:2], LN_EPS, None, ALU.add, None, [tb_mv], [tb_mv])
                act(mv[:, 3:4], mv[:, 2:3], AF.Sqrt, [tb_mv], [tb_mv])
                V(lambda: nc.vector.reciprocal(out=mv[:, 4:5], in_=mv[:, 3:4]), [tb_mv], [tb_mv])
                vts(xa[:], xa[:], mv[:, 0:1], mv[:, 4:5], ALU.subtract, ALU.mult, [tb_xa, tb_mv], [tb_xa])
                vtt(xa[:], xa[:], gt[:], ALU.mult, [tb_xa, tb_g], [tb_xa])
                vtt(xa[:], xa[:], bt[:], ALU.add, [tb_xa, tb_b], [tb_xa])
                if dst_final is not None:
                    ev = cx.dma("sp", dst_final[rs, :], xa[:], R=[tb_xa])
                    cx.last_out.append(ev)
                    continue
                cx.dma("sp", xres_d[rs, :], xa[:], R=[tb_xa], W=[tb_xres[tt]])
                A(lambda: nc.scalar.copy(out=xh[:], in_=xa[:]), [tb_xa], [tb_xh])
                transpose_tile_to_xT(xh, tb_xh, tt, st, xdst, tb_xdst)
                if router is not None:
                    for e in range(NE):
                        vtt(xb[:], xa[:], wr[:, e, :], ALU.mult, [tb_xa, tb_wr], [tb_xb])
                        V(lambda e=e: nc.vector.reduce_sum(out=lg[:, e:e + 1], in_=xb[:], axis=AX.X), [tb_xb], [tb_lg])
                    V(lambda: nc.vector.max(out=lg[:, 8:16], in_=lg[:, 0:NE]), [tb_lg], [tb_lg])
                    vts(mv[:, 5:6], lg[:, 8:9], -1.0, None, ALU.mult, None, [tb_lg], [tb_mv])
                    act(rw[:, tt, :], lg[:, 0:NE], AF.Exp, [tb_lg, tb_mv], [tb_rw], bias=mv[:, 5:6], scale=1.0)
                    act(mv[:, 6:7], lg[:, 9:10], AF.Exp, [tb_lg, tb_mv], [tb_mv], bias=mv[:, 5:6], scale=1.0)
                    vts(mv[:, 6:7], mv[:, 6:7], 1.0, None, ALU.add, None, [tb_mv], [tb_mv])
                    V(lambda: nc.vector.reciprocal(out=mv[:, 7:8], in_=mv[:, 6:7]), [tb_mv], [tb_mv])
                    vts(lg[:, 0:NE], lg[:, 0:NE], lg[:, 9:10], mv[:, 7:8], ALU.is_ge, ALU.mult, [tb_lg, tb_mv], [tb_lg])
                    vtt(rw[:, tt, :], rw[:, tt, :], lg[:, 0:NE], ALU.mult, [tb_rw, tb_lg], [tb_rw])

        def load_input_stage(st, s):
            xa, tb_xa = sb(st, "li_xa", [128, D], F32)
            xh, tb_xh = sb(st, "li_xh", [128, D], BF16)
            for tt in range(NT):
                rs = slice(tt * 128, (tt + 1) * 128)
                cx.dma("sp", xa[:], x_in[s, rs, :], W=[tb_xa])
                cx.dma("sp", xres_d[rs, :], xa[:], R=[tb_xa], W=[tb_xres[tt]])
                A(lambda: nc.scalar.copy(out=xh[:], in_=xa[:]), [tb_xa], [tb_xh])
                transpose_tile_to_xT(xh, tb_xh, tt, st)

        def rope_tables(st, pos_ap, NT, inv, tb_inv, nf, name):
            cos, tb_c = sb(st, name + "_c", [128, NT, nf], F32)
            sin, tb_s = sb(st, name + "_s", [128, NT, nf], F32)
            pi_, tb_pi = sb(st, name + "_pi", [128, NT], I32)
            pf, tb_pf = sb(st, name + "_pf", [128, NT], F32)
            k_, tb_k = sb(st, name + "_k", [128, NT, nf], F32)
            halfpi, tb_hp = sb(st, name + "_hp", [128, 1], F32)
            V(lambda: nc.vector.memset(halfpi[:], float(np.pi / 2)), [], [tb_hp])
            cx.dma("sp", pi_[:], pos_ap.rearrange("(t p) -> p t", p=128), W=[tb_pi], allow_slow_non_contiguous=True)
            V(lambda: nc.vector.tensor_copy(out=pf[:], in_=pi_[:]), [tb_pi], [tb_pf])
            for tt in range(NT):
                vts(sin[:, tt, :], inv[:, 0:nf], pf[:, tt:tt + 1], None, ALU.mult, None, [tb_inv, tb_pf], [tb_s])
            sf, kf, cf = (sin[:].rearrange("p a b -> p (a b)"), k_[:].rearrange("p a b -> p (a b)"),
                          cos[:].rearrange("p a b -> p (a b)"))
            vts(kf, sf, 1.0 / TWO_PI, MAGIC, ALU.mult, ALU.add, [tb_s], [tb_k])
            vts(kf, kf, MAGIC, None, ALU.subtract, None, [tb_k], [tb_k])
            vstt(sf, kf, -C1, sf, ALU.mult, ALU.add, [tb_k, tb_s], [tb_s])
            vstt(sf, kf, -C2, sf, ALU.mult, ALU.add, [tb_k, tb_s], [tb_s])
            vts(sf, sf, float(np.pi), float(-np.pi), ALU.min, ALU.max, [tb_s], [tb_s])
            vts(kf, sf, -1.0, None, ALU.mult, None, [tb_s], [tb_k])
            vtt(kf, kf, sf, ALU.max, [tb_k, tb_s], [tb_k])
            act(cf, kf, AF.Sin, [tb_k, tb_hp], [tb_c], bias=halfpi[:, 0:1], scale=-1.0)
            act(sf, sf, AF.Sin, [tb_s], [tb_s])
            return cos, tb_c, sin, tb_s

        def mixer(l, s, split):
            W = w_in[l]
            TQ = TQL if split else T
            NTQ, NBQ = TQ // 128, TQ // 512
            lim_t, tb_lim = (limq, tb_limq) if split else (limf, tb_limf)
            with scope() as mst:
                if split:
                    xq, tb_xq = sb(mst, "xqm", [128, KC, TQ], BF16)
                    for k0 in range(KC):
                        blend(xq[:, k0, :], xT[:, k0, :], TQ, [tb_xT], [tb_xq])
                else:
                    xq, tb_xq = xT, tb_xT
                mixer_body(l, s, split, W, TQ, NTQ, NBQ, xq, tb_xq, lim_t, tb_lim)

        def mixer_body(l, s, split, W, TQ, NTQ, NBQ, xq, tb_xq, lim_t, tb_lim):
            with scope() as st:
                kT, tb_kT = sb(st, "kT", [128, G, T], BF16)
                kiT, tb_kiT = sb(st, "kiT", [128, T], BF16)
                va, tb_va = sb(st, "va", [128, NT, G, 132], BF16)
                wi, tb_wi = sb(st, "wi", [128, NT, HI], F32)
                with scope() as sa:
                    ca, tb_ca, sa_, tb_sa = rope_tables(sa, pos_in[s], NT, inva, tb_inva, 64, "ra")
                    ci, tb_ci, si_, tb_si = rope_tables(sa, pos_in[s], NT, invi, tb_invi, 32, "ri")
                    if split:
                        caq, tb_caq, saq, tb_saq = rope_tables(sa, posq_in[s], NTQ, inva, tb_inva, 64, "rqa")
                        ciq, tb_ciq, siq, tb_siq = rope_tables(sa, posq_in[s], NTQ, invi, tb_invi, 32, "rqi")
                    else:
                        caq, tb_caq, saq, tb_saq = ca, tb_ca, sa_, tb_sa
                        ciq, tb_ciq, siq, tb_siq = ci, tb_ci, si_, tb_si
                    wb = [sb(sa, "wb%d" % i, [128, KC, 512], BF16) for i in range(2)]
                    rb = [sb(sa, "rb%d" % i, [128, 512], BF16) for i in range(2)]
                    t1, tb_t1 = sb(sa, "t1", [128, 256], F32)
                    t2, tb_t2 = sb(sa, "t2", [128, 256], F32)
                    ob = [sb(sa, "ob%d" % i, [128, 4, 128], BF16) for i in range(2)]
                    V(lambda: nc.vector.memset(va[:], 1.0), [], [tb_va])
                    blocks = []
                    for h0 in range(0, H, 4):
                        blocks.append(("q", O_Q + h0 * 128, min(4, H - h0) * 128, h0))
                    for g0 in range(0, G, 4):
                        blocks.append(("k", O_K + g0 * 128, min(4, G - g0) * 128, g0))
                    for g0 in range(0, G, 4):
                        blocks.append(("v", O_V + g0 * 128, min(4, G - g0) * 128, g0))
                    for h0 in range(0, HI, 8):
                        blocks.append(("qi", O_QI + h0 * 64, min(8, HI - h0) * 64, h0))
                    blocks.append(("ki", O_KI, 64 + HI, 0))
                    blocks.append(("wi", O_KI, 64 + HI, 0))

                    def rope(dst, src, n, half, cos, tb_c, sin, tb_s, tt, tbs_src, tb_dst):
                        nh = n // (2 * half)
                        s3 = src[:, 0:n].rearrange("p (h two d) -> p h two d", two=2, d=half)
                        d3 = dst[:, 0:n].rearrange("p (h two d) -> p h two d", two=2, d=half)
                        a3 = t1[:, 0:n // 2].rearrange("p (h d) -> p h d", d=half)
                        b3 = t2[:, 0:n // 2].rearrange("p (h d) -> p h d", d=half)
                        cb = cos[:, tt, :].unsqueeze(1).broadcast_to([128, nh, half])
                        sbb = sin[:, tt, :].unsqueeze(1).broadcast_to([128, nh, half])
                        x1, x2 = s3[:, :, 0, :], s3[:, :, 1, :]
                        vtt(a3, x1, cb, ALU.mult, tbs_src + [tb_c], [tb_t1])
                        vtt(b3, x2, sbb, ALU.mult, tbs_src + [tb_s], [tb_t2])
                        vtt(d3[:, :, 0, :], a3, b3, ALU.subtract, [tb_t1, tb_t2], [tb_dst])
                        vtt(a3, x2, cb, ALU.mult, tbs_src + [tb_c], [tb_t1])
                        vtt(b3, x1, sbb, ALU.mult, tbs_src + [tb_s], [tb_t2])
                        vtt(d3[:, :, 1, :], a3, b3, ALU.add, [tb_t1, tb_t2], [tb_dst])

                    cnt = 0
                    for bi, (kind, c0, n, i0) in enumerate(blocks):
                        wt, tb_w = wb[bi % 2]
                        wload(wt, tb_w, W[:, c0:c0 + n], D, n)
                        own = kind in ("q", "qi", "wi")
                        xs, tb_xs = (xq, tb_xq) if own else (xT, tb_xT)
                        for tt in range(NTQ if own else NT):
                            ps, tb_ps = psAB[cnt % 2]
                            r_, tb_r = rb[cnt % 2]
                            o_, tb_o = ob[cnt % 2]
                            cnt += 1
                            for k in range(KC):
                                MM(ps[:, 0:n], xs[:, k, tt * 128:(tt + 1) * 128], wt[:, k, 0:n], k == 0, k == KC - 1,
                                   [tb_xs, tb_w], [tb_ps])
                            ts = slice(tt * 128, (tt + 1) * 128)
                            if kind == "q":
                                rope(r_, ps, n, 64, caq, tb_caq, saq, tb_saq, tt, [tb_ps], tb_r)
                            if kind == "k":
                                rope(r_, ps, n, 64, ca, tb_ca, sa_, tb_sa, tt, [tb_ps], tb_r)
                            if kind in ("q", "k"):
                                nh = n // 128
                                for j in range(nh):
                                    TR(psT[:, j * 128:(j + 1) * 128], r_[:, j * 128:(j + 1) * 128], ident[:],
                                       [tb_r, tb_id], [tb_psT])
                                src = psT[:, 0:n].rearrange("p (h t) -> p h t", t=128)
                                if kind == "k":
                                    A(lambda: nc.scalar.copy(out=kT[:, i0:i0 + nh, ts], in_=src), [tb_psT], [tb_kT])
                                else:
                                    A(lambda: nc.scalar.copy(out=o_[:, 0:nh, :], in_=src), [tb_psT], [tb_o])
                                    cx.dma("sp", qT_d[tt, :, i0:i0 + nh, :], o_[:, 0:nh, :], R=[tb_o], W=[tb_qT[tt]])
                            elif kind == "v":
                                ng = n // 128
                                A(lambda: nc.scalar.copy(out=va[:, tt, i0:i0 + ng, 0:128],
                                                         in_=ps[:, 0:n].rearrange("p (g d) -> p g d", d=128)),
                                  [tb_ps], [tb_va])
                            elif kind == "qi":
                                rope(r_, ps, n, 32, ciq, tb_ciq, siq, tb_siq, tt, [tb_ps], tb_r)
                                npair = n // 128
                                for j in range(npair):
                                    TR(psT[:, j * 128:(j + 1) * 128], r_[:, j * 128:(j + 1) * 128], ident[:],
                                       [tb_r, tb_id], [tb_psT])
                                A(lambda: nc.scalar.copy(out=o_[:, 0:npair, :],
                                                         in_=psT[:, 0:npair * 128].rearrange("p (h t) -> p h t", t=128)),
                                  [tb_psT], [tb_o])
                                cx.dma("sp", qiT_d[tt, :, i0 // 2:i0 // 2 + npair, :], o_[:, 0:npair, :], R=[tb_o],
                                       W=[tb_qiT[tt]])
                            elif kind == "wi":
                                V(lambda: nc.vector.tensor_copy(out=wi[:, tt, :], in_=ps[:, 64:64 + HI]), [tb_ps], [tb_wi])
                            else:
                                rope(r_, ps, 64, 32, ci, tb_ci, si_, tb_si, tt, [tb_ps], tb_r)
                                V(lambda: nc.vector.tensor_copy(out=r_[:, 64:128], in_=r_[:, 0:64]), [tb_r], [tb_r])
                                TR(psT[:, 0:128], r_[:, 0:128], ident[:], [tb_r, tb_id], [tb_psT])
                                A(lambda: nc.scalar.copy(out=kiT[:, ts], in_=psT[:, 0:128]), [tb_psT], [tb_kiT])

                with scope() as sbk:
                    iota_s, tb_io = sb(sbk, "iota_s", [128, T], F32)
                    iota_p, tb_ip = sb(sbk, "iota_p", [128, 1], F32)
                    P(lambda: nc.gpsimd.iota(iota_s[:], pattern=[[1, T]], base=0, channel_multiplier=0,
                                             allow_small_or_imprecise_dtypes=True), [], [tb_io])
                    qt = [sb(sbk, "qt%d" % i, [128, H, 128], BF16) for i in range(2)]
                    qit = [sb(sbk, "qit%d" % i, [128, HI // 2, 128], BF16) for i in range(2)]
                    acc, tb_acc = sb(sbk, "acc", [128, T], F32)
                    wk, tb_wk = sb(sbk, "wk", [128, T], F32)
                    msk, tb_msk = sb(sbk, "msk", [128, T], BF16)
                    rl = [sb(sbk, "rl%d" % i, [128, 512], F32) for i in range(2)]
                    m8, tb_m8 = sb(sbk, "m8", [128, 8], F32)
                    sm, tb_sm = sb(sbk, "sm", [128, 8], F32)
                    ee = [sb(sbk, "ee%d" % i, [128, T], BF16) for i in range(2)]
                    pt = [sb(sbk, "pt%d" % i, [128, NT, 128], BF16) for i in range(2)]
                    ot, tb_ot = sb(sbk, "ot", [128, H, 128], BF16)
                    yo, tb_yo = sb(sbk, "yo", [128, H, 128], BF16)
                    scale = 128.0 ** -0.5
                    for tt in range(NTQ):
                        q_, tb_q = qt[tt % 2]
                        qi_, tb_qi = qit[tt % 2]
                        cx.dma("sp", q_[:], qT_d[tt], R=[tb_qT[tt]], W=[tb_q])
                        cx.dma("sp", qi_[:], qiT_d[tt], R=[tb_qiT[tt]], W=[tb_qi])
                        NKT = min(NT, (2 * tt + 2) if split else (tt + 1))
                        KW = NKT * 128
                        NKB = (KW + 511) // 512
                        vts(acc[:, 0:KW], iota_s[:, 0:KW], lim_t[:, tt:tt + 1], NEG1, ALU.is_ge, ALU.mult,
                            [tb_io, tb_lim], [tb_acc])
                        c2 = 0
                        for h in range(HI):
                            hp, ho = h // 2, (h % 2) * 64
                            for kb in range(NKB):
                                ps, tb_ps = psAB[c2 % 2]
                                r_, tb_r = rl[c2 % 2]
                                c2 += 1
                                k0_, k1_ = kb * 512, min(KW, (kb + 1) * 512)
                                w_ = k1_ - k0_
                                MM(ps[:, 0:w_], qi_[ho:ho + 64, hp, :], kiT[ho:ho + 64, k0_:k1_], True, True,
                                   [tb_qi, tb_kiT], [tb_ps])
                                act(r_[:, 0:w_], ps[:, 0:w_], AF.Relu, [tb_ps], [tb_r])
                                vstt(acc[:, k0_:k1_], r_[:, 0:w_], wi[:, tt, h:h + 1],
                                     acc[:, k0_:k1_], ALU.mult, ALU.add, [tb_r, tb_wi, tb_acc], [tb_acc])
                        if KW > TOPK:
                            cur, tb_cur = acc, tb_acc
                            for r in range(NR):
                                V(lambda cur=cur: nc.vector.max(out=m8[:], in_=cur[:, 0:KW]), [tb_cur], [tb_m8])
                                if r < NR - 1:
                                    V(lambda cur=cur: nc.vector.match_replace(out=wk[:, 0:KW], in_to_replace=m8[:],
                                                                              in_values=cur[:, 0:KW], imm_value=NEG2),
                                      [tb_cur, tb_m8], [tb_wk])
                                    cur, tb_cur = wk, tb_wk
                            vts(sm[:, 0:1], m8[:, 7:8], NEG1 * 0.1, None, ALU.max, None, [tb_m8], [tb_sm])
                            vts(msk[:, 0:KW], acc[:, 0:KW], sm[:, 0:1], None, ALU.is_ge, None, [tb_acc, tb_sm], [tb_msk])
                        else:
                            vts(msk[:, 0:KW], acc[:, 0:KW], NEG1 * 0.1, None, ALU.is_ge, None, [tb_acc], [tb_msk])
                        if DBG == "attn0" and tt == 0:
                            dbg = {}
                            for nm, shp, dt in (("dbg_acc", [128, T], F32), ("dbg_msk", [128, T], BF16), ("dbg_e", [128, T], BF16),
                                                ("dbg_sm", [128, 8], F32), ("dbg_m8", [128, 8], F32), ("dbg_ot", [128, H * 128], BF16),
                                                ("dbg_pso", [128, 132], F32), ("dbg_va", [128, NT * G * 132], BF16),
                                                ("dbg_kT", [128, G * T], BF16), ("dbg_kiT", [128, T], BF16), ("dbg_wi", [128, NT * HI], F32)):
                                dbg[nm] = nc.dram_tensor(nm, shp, dt, kind="ExternalOutput").ap()
                            evs = [cx.dma("sp", dbg["dbg_acc"], acc[:], R=[tb_acc]), cx.dma("sp", dbg["dbg_msk"], msk[:], R=[tb_msk]),
                                   cx.dma("sp", dbg["dbg_m8"], m8[:], R=[tb_m8]),
                                   cx.dma("sp", dbg["dbg_va"], va[:].rearrange("p a b c -> p (a b c)"), R=[tb_va]),
                                   cx.dma("sp", dbg["dbg_kT"], kT[:].rearrange("p a b -> p (a b)"), R=[tb_kT]),
                                   cx.dma("sp", dbg["dbg_kiT"], kiT[:], R=[tb_kiT]),
                                   cx.dma("sp", dbg["dbg_wi"], wi[:].rearrange("p a b -> p (a b)"), R=[tb_wi])]
                        for h in range(H):
                            g = h // HPG
                            e_, tb_e = ee[h % 2]
                            p_, tb_p = pt[h % 2]
                            for kb in range(NKB):
                                k0_, k1_ = kb * 512, min(KW, (kb + 1) * 512)
                                MM(psS[:, k0_:k1_], q_[:, h, :], kT[:, g, k0_:k1_], True, True,
                                   [tb_q, tb_kT], [tb_psQ[kb]])
                            V(lambda: nc.vector.reduce_max(out=sm[:, 1:2], in_=psS[:, 0:KW], axis=AX.X), tb_psQ[:NKB], [tb_sm])
                            vts(sm[:, 2:3], sm[:, 1:2], -scale, None, ALU.mult, None, [tb_sm], [tb_sm])
                            act(e_[:, 0:KW], psS[:, 0:KW], AF.Exp, tb_psQ[:NKB] + [tb_sm], [tb_e], bias=sm[:, 2:3], scale=scale)
                            vtt(e_[:, 0:KW], e_[:, 0:KW], msk[:, 0:KW], ALU.mult, [tb_e, tb_msk], [tb_e])
                            for k0 in range(0, NKT, 8):
                                k1 = min(NKT, k0 + 8)
                                for k in range(k0, k1):
                                    TR(psT[:, (k - k0) * 128:(k - k0 + 1) * 128], e_[:, k * 128:(k + 1) * 128], ident[:],
                                       [tb_e, tb_id], [tb_psT])
                                A(lambda k0=k0, k1=k1: nc.scalar.copy(
                                    out=p_[:, k0:k1, :], in_=psT[:, 0:(k1 - k0) * 128].rearrange("p (k t) -> p k t", t=128)),
                                  [tb_psT], [tb_p])
                            for k in range(NKT):
                                MM(psO[:, 0:129], p_[:, k, :], va[:, k, g, 0:129], k == 0, k == NKT - 1,
                                   [tb_p, tb_va], [tb_psO])
                            V(lambda: nc.vector.reciprocal(out=sm[:, 3:4], in_=psO[:, 128:129]), [tb_psO], [tb_sm])
                            if DBG == "attn0" and tt == 0 and h == 0:
                                V(lambda: nc.vector.tensor_copy(out=wk[:, 0:132], in_=psO[:, 0:132]), [tb_psO], [tb_wk])
                                evs += [cx.dma("sp", dbg["dbg_e"], e_[:], R=[tb_e]), cx.dma("sp", dbg["dbg_sm"], sm[:], R=[tb_sm]),
                                        cx.dma("sp", dbg["dbg_pso"], wk[:, 0:132], R=[tb_wk])]
                            vts(ot[:, h, :], psO[:, 0:128], sm[:, 3:4], None, ALU.mult, None, [tb_psO, tb_sm], [tb_ot])
                        if DBG == "attn0" and tt == 0:
                            evs.append(cx.dma("sp", dbg["dbg_ot"], ot[:].rearrange("p a b -> p (a b)"), R=[tb_ot]))
                            for ev in evs:
                                cx._wait("sp", ev)
                            stopf[0] = True
                            break
                        for h0 in range(0, H, 8):
                            h1 = min(H, h0 + 8)
                            for h in range(h0, h1):
                                TR(psT[:, (h - h0) * 128:(h - h0 + 1) * 128], ot[:, h, :], ident[:], [tb_ot, tb_id], [tb_psT])
                            A(lambda h0=h0, h1=h1: nc.scalar.copy(
                                out=yo[:, h0:h1, :], in_=psT[:, 0:(h1 - h0) * 128].rearrange("p (k t) -> p k t", t=128)),
                              [tb_psT], [tb_yo])
                        cx.dma("sp", yT_d[0:NAC, :, tt * 128:(tt + 1) * 128].rearrange("h p t -> p h t"), yo[:],
                               R=[tb_yo], W=tb_yT[0:NAC])

            if DBG == "attnB":
                stopf[0] = True
            if stopf[0]:
                return
            with scope() as st:
                wc = [sb(st, "wc%d" % i, [128, KC, 128], BF16) for i in range(6)]
                wcn = [0]

                def proj_fm(c0, dst, tb_dst, func=None, own=False):
                    wt, tb_w = wc[wcn[0] % 6]
                    wcn[0] += 1
                    wload(wt, tb_w, W[:, c0:c0 + 128], D, 128)
                    xs, tb_xs = (xq, tb_xq) if own else (xT, tb_xT)
                    for tb_ in range(NBQ if own else NB):
                        ps, tb_ps = psQ[tb_]
                        for k in range(KC):
                            MM(ps, wt[:, k, :], xs[:, k, tb_ * 512:(tb_ + 1) * 512], k == 0, k == KC - 1,
                               [tb_w, tb_xs], [tb_ps])
                        if func is None:
                            A(lambda tb_=tb_, ps=ps: nc.scalar.copy(out=dst[:, tb_ * 512:(tb_ + 1) * 512], in_=ps),
                              [tb_ps], [tb_dst])
                        else:
                            act(dst[:, tb_ * 512:(tb_ + 1) * 512], ps, func, [tb_ps], [tb_dst])

                f = [sb(st, "f%d" % i, [128, T], F32) for i in range(7)]
                hb = [sb(st, "hb%d" % i, [128, T], BF16) for i in range(4)]
                yb, tb_yb = sb(st, "yb", [128, T], BF16)
                col, tb_col = sb(st, "col", [128, 16], F32)
                cw, tb_cw = sb(st, "cw", [128, 4, 128], BF16)
                wa_t, tb_wa = sb(st, "wa_t", [128, 1, 128], BF16)
                wx_t, tb_wx = sb(st, "wx_t", [128, 1, 128], BF16)

                def colvec(j, src_row, c0):
                    cx.dma("sp", col[:, j:j + 1], src_row[c0:c0 + 128].rearrange("(p o) -> p o", o=1), W=[tb_col],
                           allow_slow_non_contiguous=True)

                for g in range(4):
                    win = (2, 4, 8, 16)[g]
                    ncg = PW // 128
                    for i in range(ncg):
                        c = g * ncg + i
                        (p_, tb_p), (a_, tb_a), (b_, tb_b) = f[0], f[1], f[2]
                        proj_fm(O_P + c * 128, p_, tb_p)
                        src, tb_src = p_, tb_p
                        sh = 1
                        flip = 0
                        while sh < win:
                            dst, tb_dst = (a_, tb_a) if flip == 0 else (b_, tb_b)
                            flip ^= 1
                            V(lambda dst=dst, src=src, sh=sh: nc.vector.tensor_copy(out=dst[:, 0:sh], in_=src[:, 0:sh]),
                              [tb_src], [tb_dst])
                            vtt(dst[:, sh:T], src[:, sh:T], src[:, 0:T - sh], ALU.add, [tb_src], [tb_dst])
                            src, tb_src = dst, tb_dst
                            sh *= 2
                        d_, tb_d = (a_, tb_a) if src is b_ else (b_, tb_b)
                        vstt(d_[:, win - 1:T], src[:, win - 1:T], 1.0 / win, p_[:, win - 1:T], ALU.mult, ALU.subtract,
                             [tb_src, tb_p], [tb_d])
                        for t in range(win - 1):
                            vstt(d_[:, t:t + 1], src[:, t:t + 1], 1.0 / (t + 1), p_[:, t:t + 1], ALU.mult, ALU.subtract,
                                 [tb_src, tb_p], [tb_d])
                        if split:
                            blend(hb[i][0][:, 0:TQ], d_[:], TQ, [tb_d], [hb[i][1]])
                        else:
                            V(lambda d_=d_, i=i: nc.vector.tensor_copy(out=hb[i][0][:], in_=d_[:]), [tb_d], [hb[i][1]])
                    for j in range(ncg):
                        c = g * ncg + j
                        wload(cw, tb_cw, pool_w[l, g][:, j * 128:(j + 1) * 128], PW, 128)
                        colvec(0, pool_scale[l], c * 128)
                        gt_, tb_gt = f[3]
                        proj_fm(O_GT + (NAC + c) * 128, gt_, tb_gt, AF.Sigmoid, own=True)
                        for tb_ in range(NBQ):
                            ps, tb_ps = psAB[tb_ % 2]
                            for i in range(ncg):
                                MM(ps[:, :], cw[:, i, :], hb[i][0][:, tb_ * 512:(tb_ + 1) * 512], i == 0, i == ncg - 1,
                                   [tb_cw, hb[i][1]], [tb_ps])
                            vstt(yb[:, tb_ * 512:(tb_ + 1) * 512], ps[:, :], col[:, 0:1], gt_[:, tb_ * 512:(tb_ + 1) * 512],
                                 ALU.mult, ALU.mult, [tb_ps, tb_col, tb_gt], [tb_yb])
                        cx.dma("sp", yT_d[NAC + c][:, 0:TQ], yb[:, 0:TQ], R=[tb_yb], W=[tb_yT[NAC + c]])
                for n in range(KC):
                    (xr, tb_xr), (xc, tb_xc), (r_, tb_r), (i_, tb_i), (a_, tb_a), (g_, tb_g), (gt_, tb_gt) = f
                    xcb, tb_xcb = hb[0]
                    proj_fm(O_XR + n * 128, xr, tb_xr)
                    for tap in range(4):
                        colvec(tap, conv_w[l, tap], n * 128)
                    colvec(4, conv_b[l], n * 128)
                    colvec(5, lru_ba[l], n * 128)
                    colvec(6, lru_bx[l], n * 128)
                    colvec(7, lru_lam[l], n * 128)
                    act(col[:, 8:9], col[:, 7:8], AF.Exp, [tb_col], [tb_col], scale=-1.0)
                    act(col[:, 9:10], col[:, 8:9], AF.Ln, [tb_col], [tb_col], bias=1.0, scale=1.0)
                    vts(col[:, 10:11], col[:, 9:10], -8.0, None, ALU.mult, None, [tb_col], [tb_col])
                    vts(xc[:], xr[:], col[:, 3:4], col[:, 4:5], ALU.mult, ALU.add, [tb_xr, tb_col], [tb_xc])
                    for tap in range(3):
                        sh = 3 - tap
                        vstt(xc[:, sh:T], xr[:, 0:T - sh], col[:, tap:tap + 1], xc[:, sh:T], ALU.mult, ALU.add,
                             [tb_xr, tb_col, tb_xc], [tb_xc])
                    V(lambda: nc.vector.tensor_copy(out=xcb[:], in_=xc[:]), [tb_xc], [tb_xcb])
                    wload(wa_t, tb_wa, lru_wa[l, n], 128, 128)
                    wload(wx_t, tb_wx, lru_wx[l, n], 128, 128)
                    for tb_ in range(NB):
                        tsl = slice(tb_ * 512, (tb_ + 1) * 512)
                        psa, tb_psa = psAB[0]
                        psb, tb_psb = psAB[1]
                        MM(psa[:, :], wa_t[:, 0, :], xcb[:, tsl], True, True, [tb_wa, tb_xcb], [tb_psa])
                        MM(psb[:, :], wx_t[:, 0, :], xcb[:, tsl], True, True, [tb_wx, tb_xcb], [tb_psb])
                        act(r_[:, tsl], psa[:, :], AF.Sigmoid, [tb_psa, tb_col], [tb_r], bias=col[:, 5:6], scale=1.0)
                        act(i_[:, tsl], psb[:, :], AF.Sigmoid, [tb_psb, tb_col], [tb_i], bias=col[:, 6:7], scale=1.0)
                    act(a_[:], r_[:], AF.Exp, [tb_r, tb_col], [tb_a], scale=col[:, 10:11])
                    vtt(r_[:], a_[:], a_[:], ALU.mult, [tb_a], [tb_r])
                    vts(r_[:], r_[:], -1.0, 1.0, ALU.mult, ALU.add, [tb_r], [tb_r])
                    vts(r_[:], r_[:], 0.0, None, ALU.max, None, [tb_r], [tb_r])
                    act(r_[:], r_[:], AF.Sqrt, [tb_r], [tb_r])
                    vtt(i_[:], i_[:], xc[:], ALU.mult, [tb_i, tb_xc], [tb_i])
                    vtt(i_[:], i_[:], r_[:], ALU.mult, [tb_i, tb_r], [tb_i])
                    V(lambda: nc.vector.tensor_tensor_scan(out=xc[:], data0=a_[:], data1=i_[:], initial=0.0,
                                                           op0=ALU.mult, op1=ALU.add), [tb_a, tb_i], [tb_xc])
                    if split:
                        blend(a_[:, 0:TQ], xc[:], TQ, [tb_xc], [tb_a])
                        hq, tb_hq = a_, tb_a
                    else:
                        hq, tb_hq = xc, tb_xc
                    q_ = slice(0, TQ)
                    proj_fm(O_GR + n * 128, g_, tb_g, own=True)
                    vtt(r_[:, q_], g_[:, q_], g_[:, q_], ALU.mult, [tb_g], [tb_r])
                    vts(r_[:, q_], r_[:, q_], 0.044715, 1.0, ALU.mult, ALU.add, [tb_r], [tb_r])
                    vtt(r_[:, q_], r_[:, q_], g_[:, q_], ALU.mult, [tb_r, tb_g], [tb_r])
                    act(r_[:, q_], r_[:, q_], AF.Sigmoid, [tb_r], [tb_r], scale=float(2.0 * np.sqrt(2.0 / np.pi)))
                    vtt(r_[:, q_], r_[:, q_], g_[:, q_], ALU.mult, [tb_r, tb_g], [tb_r])
                    vtt(r_[:, q_], r_[:, q_], hq[:, q_], ALU.mult, [tb_r, tb_hq], [tb_r])
                    proj_fm(O_GT + (NAC + KC + n) * 128, gt_, tb_gt, AF.Sigmoid, own=True)
                    vtt(yb[:, q_], r_[:, q_], gt_[:, q_], ALU.mult, [tb_r, tb_gt], [tb_yb])
                    cx.dma("sp", yT_d[NAC + KC + n][:, q_], yb[:, q_], R=[tb_yb], W=[tb_yT[NAC + KC + n]])
                for c in range(NAC):
                    gt_, tb_gt = f[3]
                    ya, tb_ya = hb[c % 2]
                    proj_fm(O_GT + c * 128, gt_, tb_gt, AF.Sigmoid, own=True)
                    cx.dma("sp", ya[:, 0:TQ], yT_d[c][:, 0:TQ], R=[tb_yT[c]], W=[tb_ya])
                    vtt(yb[:, 0:TQ], ya[:, 0:TQ], gt_[:, 0:TQ], ALU.mult, [tb_ya, tb_gt], [tb_yb])
                    cx.dma("sp", yT_d[c][:, 0:TQ], yb[:, 0:TQ], R=[tb_yb], W=[tb_yT[c]])
                for c in range(NMC):
                    ya, tb_ya = hb[c % 4]
                    cx.dma("sp", ya[:, 0:TQ], yT_d[c][:, 0:TQ], R=[tb_yT[c]], W=[tb_ya])
                    cx.dma("sp", mT_d[0:NTQ, :, c, :].rearrange("t p q -> p t q"),
                           ya[:, 0:TQ].rearrange("p (t q) -> p t q", q=128), R=[tb_ya], W=[tb_mT])

            with scope() as st:
                wo, tb_wo = sb(st, "wo", [128, NMC, 512], BF16)
                mt = [sb(st, "mt%d" % i, [128, NMC, 128], BF16) for i in range(2)]
                po = [sb(st, "po%d" % i, [128, 512], F32) for i in range(2)]
                for nb in range(D // 512):
                    wload(wo, tb_wo, w_out[l][:, nb * 512:(nb + 1) * 512], MIXW, 512)
                    for tt in range(NTQ):
                        m_, tb_m = mt[tt % 2]
                        o_, tb_o = po[tt % 2]
                        ps, tb_ps = psAB[tt % 2]
                        cx.dma("sp", m_[:], mT_d[tt], R=[tb_mT], W=[tb_m])
                        for c in range(NMC):
                            MM(ps[:, :], m_[:, c, :], wo[:, c, :], c == 0, c == NMC - 1, [tb_m, tb_wo], [tb_ps])
                        A(lambda o_=o_, ps=ps: nc.scalar.copy(out=o_[:], in_=ps[:, :]), [tb_ps], [tb_o])
                        cx.dma("sp", pre_d[tt * 128:(tt + 1) * 128, nb * 512:(nb + 1) * 512], o_[:], R=[tb_o],
                               W=[tb_pre[tt]])

        def ffn(wg, wu, wd, F, first, scale_e, xq, tb_xq, TQ, GF, yacc=None, tb_yacc=None):
            nfc = (F + 127) // 128
            assert F % 128 == 0 and GF % 4 == 0
            NTQ, NBQ = TQ // 128, TQ // 512
            with scope() as st:
                wgc = [sb(st, "wgc%d" % i, [128, KC, 512], BF16) for i in range(2)]
                wuc = [sb(st, "wuc%d" % i, [128, KC, 512], BF16) for i in range(2)]
                hT, tb_hT = sb(st, "hT", [128, GF, TQ], BF16)
                wdb = [sb(st, "wdb%d" % i, [128, GF, 512], BF16) for i in range(2)]
                sg = [sb(st, "sg%d" % i, [128, 512], F32) for i in range(2)]
                po = [sb(st, "fpo%d" % i, [128, 512], F32) for i in range(2)]
                c3 = [0]
                items = []
                hcnt = 0
                for f0 in range(0, nfc, GF):
                    f1 = min(nfc, f0 + GF)
                    for fb in range(f0, f1, 4):
                        items.append(("H", f0, f1, fb, hcnt))
                        hcnt += 1
                    for nb in range(D // 512):
                        items.append(("D", f0, f1, nb, 0))

                def load(it):
                    kind, f0, f1, j, hc = it
                    if kind == "H":
                        n = (min(f1, j + 4) - j) * 128
                        g_, tb_g = wgc[hc % 2]
                        u_, tb_u = wuc[hc % 2]
                        wload(g_, tb_g, wg[:, j * 128:j * 128 + n], D, n)
                        wload(u_, tb_u, wu[:, j * 128:j * 128 + n], D, n)
                    else:
                        w_, tb_w = wdb[j % 2]
                        wload(w_, tb_w, wd[f0 * 128:f1 * 128, j * 512:(j + 1) * 512], (f1 - f0) * 128, 512)

                def compute(it):
                    kind, f0, f1, j, hc = it
                    if kind == "H":
                        g_, tb_g = wgc[hc % 2]
                        u_, tb_u = wuc[hc % 2]
                        for fi in range(j, min(f1, j + 4)):
                            cs = slice((fi - j) * 128, (fi - j + 1) * 128)
                            for tb_ in range(NBQ):
                                tsl = slice(tb_ * 512, (tb_ + 1) * 512)
                                psa, tb_psa = psQ[(2 * tb_) % 4]
                                psb, tb_psb = psQ[(2 * tb_ + 1) % 4]
                                s_, tb_s = sg[tb_ % 2]
                                for k in range(KC):
                                    MM(psa, g_[:, k, cs], xq[:, k, tsl], k == 0, k == KC - 1, [tb_g, tb_xq], [tb_psa])
                                for k in range(KC):
                                    MM(psb, u_[:, k, cs], xq[:, k, tsl], k == 0, k == KC - 1, [tb_u, tb_xq], [tb_psb])
                                act(s_[:, :], psa, AF.Silu, [tb_psa], [tb_s])
                                vtt(hT[:, fi - f0, tsl], s_[:, :], psb, ALU.mult, [tb_s, tb_psb], [tb_hT])
                    else:
                        nb = j
                        w_, tb_w = wdb[nb % 2]
                        for tt in range(NTQ):
                            ps, tb_ps = psAB[c3[0] % 2]
                            o_, tb_o = po[c3[0] % 2]
                            c3[0] += 1
                            for fi in range(f0, f1):
                                MM(ps[:, :], hT[:, fi - f0, tt * 128:(tt + 1) * 128], w_[:, fi - f0, :], fi == f0,
                                   fi == f1 - 1, [tb_hT, tb_w], [tb_ps])
                            if yacc is not None:
                                dv = yacc[:, tt, nb * 512:(nb + 1) * 512]
                                if first and f0 == 0:
                                    if scale_e is None:
                                        A(lambda dv=dv, ps=ps: nc.scalar.copy(out=dv, in_=ps[:, :]), [tb_ps], [tb_yacc[tt]])
                                    else:
                                        vts(dv, ps[:, :], rw[:, tt, scale_e:scale_e + 1], None, ALU.mult, None,
                                            [tb_ps, tb_rw], [tb_yacc[tt]])
                                elif scale_e is None:
                                    vtt(dv, dv, ps[:, :], ALU.add, [tb_ps, tb_yacc[tt]], [tb_yacc[tt]])
                                else:
                                    vstt(dv, ps[:, :], rw[:, tt, scale_e:scale_e + 1], dv, ALU.mult, ALU.add,
                                         [tb_ps, tb_rw, tb_yacc[tt]], [tb_yacc[tt]])
                                continue
                            if scale_e is None:
                                A(lambda o_=o_, ps=ps: nc.scalar.copy(out=o_[:], in_=ps[:, :]), [tb_ps], [tb_o])
                            else:
                                vts(o_[:], ps[:, :], rw[:, tt, scale_e:scale_e + 1], None, ALU.mult, None,
                                    [tb_ps, tb_rw], [tb_o])
                            dst = yf_d[tt * 128:(tt + 1) * 128, nb * 512:(nb + 1) * 512]
                            if first and f0 == 0:
                                cx.dma("pool", dst, o_[:], R=[tb_o], W=[tb_yf[tt]])
                            else:
                                cx.dma("pool", dst, o_[:], R=[tb_o], W=[tb_yf[tt]], accum_op=ALU.add)

                load(items[0])
                for ii, it in enumerate(items):
                    if ii + 1 < len(items):
                        load(items[ii + 1])
                    compute(it)


        def drain_all():
            for tbl in (tb_xres, tb_pre, tb_yf, tb_yT, tb_qT, tb_qiT, [tb_mT]):
                for b in tbl:
                    for ev in list(b.w.values()):
                        cx._wait("sp", ev)

        for s in range(NSEQ):
            with scope() as st:
                load_input_stage(st, s)
            for l in range(L):
                split = SPLIT > 1 and l == L - 1
                ntq_l = (TQL // 128) if split else NT
                mixer(l, s, split)
                if stopf[0]:
                    drain_all()
                    return nc
                if DBG == "mixer%d" % l:
                    drain_all()
                    return nc
                with contextlib.ExitStack() as tl:
                    if split:
                        cx.barrier()
                        xst.close()
                        xs_, tb_xs_ = sb(tl, "xq2", [128, KC, TQL], BF16)
                        yacc, _ = sb(tl, "yacc", [128, TQL // 128, D], F32)
                        tb_yacc = [TB() for _ in range(TQL // 128)]
                        tq_l, gf_l = TQL, 8
                    else:
                        xs_, tb_xs_ = xT, tb_xT
                        yacc, tb_yacc = None, None
                        tq_l, gf_l = T, 8
                    with scope() as st:
                        layer_norm_stage(st, "pre", ln_mix_g[l], ln_mix_b[l],
                                         router=(routerT[l // 2] if l % 2 == 1 else None), ntq=ntq_l, res_blend=split,
                                         xdst=xs_, tb_xdst=tb_xs_)
                    if l % 2 == 0:
                        ffn(dw_gate[l // 2], dw_up[l // 2], dw_down[l // 2], DFF, True, None, xs_, tb_xs_, tq_l, gf_l,
                            yacc, tb_yacc)
                    else:
                        for e in range(NE):
                            ffn(mw_gate[l // 2, e], mw_up[l // 2, e], mw_down[l // 2, e], DEXP, e == 0, e,
                                xs_, tb_xs_, tq_l, gf_l, yacc, tb_yacc)
                    with scope() as st:
                        layer_norm_stage(st, "yf", ln_ffn_g[l], ln_ffn_b[l],
                                         dst_final=(y_out[s] if l == L - 1 else None), ntq=ntq_l,
                                         add_sb=yacc, tb_add_sb=tb_yacc)
                    cx.barrier()
        for ev in cx.last_out:
            cx._wait("sp", ev)
        xst.close()
    return nc


def consts():
    ident = np.eye(128, dtype=np.float32)
    inva = np.tile((10000.0 ** (-np.arange(0, 128, 2, dtype=np.float32) / 128.0)).astype(np.float32)[None, :], (128, 1))
    invi = np.tile((10000.0 ** (-np.arange(0, 64, 2, dtype=np.float32) / 64.0)).astype(np.float32)[None, :], (128, 1))
    return ident, inva, invi


def run(cfg, inputs):
    ncores = cfg["NCORES"]
    split = cfg.get("SPLIT", 1)
    nseq = cfg["B"] * split // ncores
    T = cfg["T"]
    tq = T // split
    nt = T // 128
    nc = build_nc(cfg)
    ident, inva, invi = consts()
    rows = np.arange(128)[:, None] + 128 * np.arange(nt)[None, :]
    limf = ((rows // 64 + 1) * 64).astype(np.float32)
    in_maps = []
    for c in range(ncores):
        b0, hf = (c // split) * nseq, c % split
        m = {}
        for k, v in inputs.items():
            v = np.asarray(v)
            if k in ("x", "positions"):
                m[k] = np.ascontiguousarray(v[b0:b0 + nseq])
            elif k == "moe_router":
                m["moe_routerT"] = np.ascontiguousarray(np.swapaxes(v, 1, 2))
            else:
                m[k] = v
        pos = np.asarray(inputs["positions"])[b0:b0 + nseq]
        if split == 1:
            m["positions_q"] = np.ascontiguousarray(pos)
        else:
            m["positions_q"] = np.ascontiguousarray(pos.reshape(nseq, nt // split, split, 128)[:, :, hf, :].reshape(nseq, tq))
        sel = np.zeros((128, 2), np.float32)
        sel[:, hf] = 1.0
        m["c_sel"] = sel
        m["c_limf"] = limf
        gl = (np.arange(nt)[None, :] * split + hf) * 128 + np.arange(128)[:, None]
        m["c_limq"] = ((gl // 64 + 1) * 64).astype(np.float32)
        m["c_ident"], m["c_inva"], m["c_invi"] = ident, inva, invi
        in_maps.append(m)
    if cfg.get("ONLY_MAPS"):
        return nc, in_maps
    if cfg.get("SUBSET") is not None:
        sub = cfg["SUBSET"]
        r = run_bass_kernel_spmd(nc, [in_maps[c] for c in sub], core_ids=list(range(len(sub))))
        return r.results
    res = run_bass_kernel_spmd(nc, in_maps, core_ids=list(range(ncores)))
    if cfg.get("STOP"):
        return res.results
    out = np.empty((cfg["B"], T, cfg["D"]), np.float32)
    for c in range(ncores):
        b0, hf = (c // split) * nseq, c % split
        if split == 1:
            out[b0:b0 + nseq] = res.results[c]["y"]
        else:
            out[b0].reshape(nt // split, split, 128, cfg["D"])[:, hf] = res.results[c]["y"][0].reshape(nt // split, 128, cfg["D"])
    return out


def kernel(**inputs):
    return run(FULL, inputs).astype(np.float32)
```
